# Optimizing a Trainium2 kernel written in Bass

```python
import jax, jax.numpy as jnp
from jax import lax
import numpy as np

D_MODEL = 4096
BATCH = 4
SEQ = 4096
DEPTH = 2

GRID_W = 64
CTX_LEN = 256
EPS = 1e-6
NEG_INF = -1e30
N_MOD = 6

MIX_W = D_MODEL
GLA_HEADS = 4
GLA_V_W = MIX_W // 2
GLA_DV = GLA_V_W // GLA_HEADS
GLA_K_W = GLA_V_W // 2
GLA_DK = GLA_K_W // GLA_HEADS
GLA_RANK = 16
GLA_TAU = 16.0
GLA_CHUNK = 64
SG_GROUPS = 4
SG_W = MIX_W // 2
SG_GC = SG_W // SG_GROUPS
SG_CHUNK = 128
AB_WIDTHS = (GLA_K_W, GLA_K_W, GLA_V_W, GLA_V_W, GLA_RANK, GLA_RANK, SG_W, SG_W)
AB_IN_W = sum(AB_WIDTHS)
ATT_HEADS = D_MODEL // 128
KV_HEADS = 8
HEAD_DIM = 128
GQ = ATT_HEADS // KV_HEADS
WINDOW = 128
ATT_BLOCK = 128
BAND = ATT_BLOCK + 2 * WINDOW
AXIS_DIM = HEAD_DIM // 2
ROPE_BASE = 10000.0
Q_W = ATT_HEADS * HEAD_DIM
KV_W = KV_HEADS * HEAD_DIM
C_IN_W = Q_W + 2 * KV_W
N_EXPERTS = 16
EXPERT_FF = D_MODEL // 4
EC_CAPACITY = 2

kernel_name = 'hybrid_gla_sgmlp_swa_ecmoe_dit'


def rms_norm(x, gain):
    xf = x.astype(jnp.float32)
    y = xf * lax.rsqrt(jnp.mean(xf * xf, axis=-1, keepdims=True) + EPS)
    return (y * gain.astype(jnp.float32)).astype(x.dtype)


def modulate(x, gain, shift, scale):
    return rms_norm(x, gain) * (1 + scale) + shift


def split_cols(p, widths):
    return jnp.split(p, [int(i) for i in np.cumsum(widths)[:-1]], axis=-1)


def gla_chunked(q, k, v, log_a, s0):
    B, T, H, dk = q.shape
    dv = v.shape[-1]
    n = T // GLA_CHUNK
    shp = lambda a: a.astype(jnp.float32).reshape(B, n, GLA_CHUNK, H, a.shape[-1])
    q, k, v, log_a = shp(q), shp(k), shp(v), shp(log_a)
    b = jnp.cumsum(log_a, axis=2)
    b_last = b[:, :, -1:]
    q_in = q * jnp.exp(b)
    k_in = k * jnp.exp(-b)
    k_dec = k * jnp.exp(b_last - b)
    causal = jnp.tril(jnp.ones((GLA_CHUNK, GLA_CHUNK), bool))
    scores = jnp.where(causal, jnp.einsum('bnihd,bnjhd->bnhij', q_in, k_in), 0.0)
    o_intra = jnp.einsum('bnhij,bnjhe->bnihe', scores, v)
    decay = jnp.exp(b_last[:, :, 0])

    def step(S, xs):
        qc, kc, vc, dc = xs
        o = jnp.einsum('bihd,bhde->bihe', qc, S)
        S = dc[..., None] * S + jnp.einsum('bjhd,bjhe->bhde', kc, vc)
        return S, o

    xs = tuple(jnp.moveaxis(a, 1, 0) for a in (q_in, k_dec, v, decay))
    S, o_inter = lax.scan(step, s0.astype(jnp.float32), xs)
    o = o_intra + jnp.moveaxis(o_inter, 0, 1)
    return o.reshape(B, T, H, dv), S


def gla_bidir(q, k, v, la_f, la_b, s_f, s_b):
    rev = lambda a: jnp.flip(a, axis=1)
    o_f, s_f = gla_chunked(q, k, v, la_f, s_f)
    o_b, s_b = gla_chunked(rev(q), rev(k), rev(v), rev(la_b), s_b)
    return o_f + rev(o_b), s_f, s_b


def gla_output(o, g, gain):
    B, T = o.shape[:2]
    y = o * lax.rsqrt(jnp.mean(o * o, axis=-1, keepdims=True) + EPS)
    y = y * gain.astype(jnp.float32).reshape(GLA_HEADS, GLA_DV)
    return y.reshape(B, T, GLA_V_W).astype(g.dtype) * jax.nn.silu(g)


def spatial_gate(u, vs, nw, nb, ws, bs):
    B, T, _ = u.shape
    u = jax.nn.gelu(u)
    vf = jax.nn.gelu(vs).astype(jnp.float32).reshape(B, T, SG_GROUPS, SG_GC)
    mu = jnp.mean(vf, axis=-1, keepdims=True)
    var = jnp.mean(jnp.square(vf - mu), axis=-1, keepdims=True)
    vn = (vf - mu) * lax.rsqrt(var + EPS) * nw.astype(jnp.float32).reshape(SG_GROUPS, SG_GC) + nb.astype(jnp.float32).reshape(SG_GROUPS, SG_GC)
    vn = vn.astype(u.dtype).reshape(B, T // SG_CHUNK, SG_CHUNK, SG_GROUPS, SG_GC)
    s = jnp.einsum('gij,bnjgc->bnigc', ws, vn) + bs.T[:, :, None]
    return u * s.reshape(B, T, SG_W)


def ab_project(x, w_in, gate_w, gate_b):
    B, T, _ = x.shape
    q, k, v, g, lr_f, lr_b, u, vs = split_cols(x @ w_in, AB_WIDTHS)
    heads = lambda a, d: a.reshape(B, T, GLA_HEADS, d)
    log_decay = lambda lr, d: jax.nn.log_sigmoid((lr @ gate_w[d] + gate_b[d]).astype(jnp.float32)) / GLA_TAU
    gla_in = (heads(q, GLA_DK) * GLA_DK ** -0.5, heads(k, GLA_DK), heads(v, GLA_DV),
              heads(log_decay(lr_f, 0), GLA_DK), heads(log_decay(lr_b, 1), GLA_DK))
    return gla_in, g, u, vs


def ab_mixer(xl, xc, w_in, gate_w, gate_b, gla_gain, sg_nw, sg_nb, sg_ws, sg_bs, w_out):
    gla_c, g_c, u_c, vs_c = ab_project(xc, w_in, gate_w, gate_b)
    gla_l, g_l, u_l, vs_l = ab_project(xl, w_in, gate_w, gate_b)
    s0 = jnp.zeros((xc.shape[0], GLA_HEADS, GLA_DK, GLA_DV), jnp.float32)
    o_c, s_f, s_b = gla_bidir(*gla_c, s0, s0)
    o_l, _, _ = gla_bidir(*gla_l, s_f, s_b)

    def merge(o, g, u, vs):
        a = gla_output(o, g, gla_gain)
        b = spatial_gate(u, vs, sg_nw, sg_nb, sg_ws, sg_bs)
        return jnp.concatenate([a, b], axis=-1) @ w_out

    return merge(o_l, g_l, u_l, vs_l), merge(o_c, g_c, u_c, vs_c)


def axial_rope_tables(T, dtype):
    rows = T // GRID_W
    row, col = jnp.meshgrid(jnp.arange(rows), jnp.arange(GRID_W), indexing='ij')
    inv_freq = ROPE_BASE ** (-jnp.arange(0, AXIS_DIM, 2, dtype=jnp.float32) / AXIS_DIM)

    def angles(pos):
        ang = pos.reshape(-1).astype(jnp.float32)[:, None] * inv_freq
        return jnp.concatenate([ang, ang], axis=-1)

    ang = jnp.concatenate([angles(row), angles(col)], axis=-1)
    return jnp.cos(ang).astype(dtype), jnp.sin(ang).astype(dtype)


def apply_axial_rope(x, cos, sin):
    def rot(p):
        p1, p2 = jnp.split(p, 2, axis=-1)
        return jnp.concatenate([-p2, p1], axis=-1)
    x_row, x_col = jnp.split(x, 2, axis=-1)
    return x * cos[:, None] + jnp.concatenate([rot(x_row), rot(x_col)], axis=-1) * sin[:, None]


def window_sink_attention(q, k, v, kc, vc, sinks):
    B, T = q.shape[:2]
    n = T // ATT_BLOCK
    scale = HEAD_DIM ** -0.5
    qb = q.reshape(B, n, ATT_BLOCK, KV_HEADS, GQ, HEAD_DIM).transpose(1, 0, 2, 3, 4, 5)
    pad = ((0, 0), (WINDOW, WINDOW), (0, 0), (0, 0))
    k_pad, v_pad = jnp.pad(k, pad), jnp.pad(v, pad)
    sink_logit = sinks.astype(jnp.float32).reshape(1, KV_HEADS, GQ, 1, 1)
    rel = jnp.arange(BAND)[None, :] - jnp.arange(ATT_BLOCK)[:, None]
    in_window = (rel >= 0) & (rel <= 2 * WINDOW)

    def block(args):
        qi, idx = args
        start = idx * ATT_BLOCK
        kb = lax.dynamic_slice_in_dim(k_pad, start, BAND, axis=1)
        vb = lax.dynamic_slice_in_dim(v_pad, start, BAND, axis=1)
        key_pos = start - WINDOW + jnp.arange(BAND)
        valid = in_window & ((key_pos >= 0) & (key_pos < T))[None, :]
        s_loc = jnp.einsum('bihgd,bjhd->bhgij', qi, kb).astype(jnp.float32) * scale
        s_loc = jnp.where(valid, s_loc, NEG_INF)
        s_ctx = jnp.einsum('bihgd,bjhd->bhgij', qi, kc).astype(jnp.float32) * scale
        sink = jnp.broadcast_to(sink_logit, s_loc.shape[:-1] + (1,))
        p = jax.nn.softmax(jnp.concatenate([s_loc, s_ctx, sink], axis=-1), axis=-1).astype(v.dtype)
        L = kc.shape[1]
        return (jnp.einsum('bhgij,bjhd->bihgd', p[..., :BAND], vb)
                + jnp.einsum('bhgij,bjhd->bihgd', p[..., BAND:BAND + L], vc))

    o = lax.map(block, (qb, jnp.arange(n)))
    return o.transpose(1, 0, 2, 3, 4, 5).reshape(B, T, Q_W)


def ctx_sink_attention(qc, kc, vc, sinks):
    B, L = qc.shape[:2]
    qg = qc.reshape(B, L, KV_HEADS, GQ, HEAD_DIM)
    s = jnp.einsum('bihgd,bjhd->bhgij', qg, kc).astype(jnp.float32) * HEAD_DIM ** -0.5
    sink = jnp.broadcast_to(sinks.astype(jnp.float32).reshape(1, KV_HEADS, GQ, 1, 1), s.shape[:-1] + (1,))
    p = jax.nn.softmax(jnp.concatenate([s, sink], axis=-1), axis=-1)[..., :-1].astype(vc.dtype)
    return jnp.einsum('bhgij,bjhd->bihgd', p, vc).reshape(B, L, Q_W)


def c_mixer(xl, xc, w_in, sinks, w_out, cos, sin, need_ctx):
    B, T, _ = xl.shape
    L = xc.shape[1]
    q, k, v = split_cols(xl @ w_in, (Q_W, KV_W, KV_W))
    q = apply_axial_rope(q.reshape(B, T, ATT_HEADS, HEAD_DIM), cos, sin)
    k = apply_axial_rope(k.reshape(B, T, KV_HEADS, HEAD_DIM), cos, sin)
    v = v.reshape(B, T, KV_HEADS, HEAD_DIM)
    kc, vc = split_cols(xc @ w_in[:, Q_W:], (KV_W, KV_W))
    kc = kc.reshape(B, L, KV_HEADS, HEAD_DIM)
    vc = vc.reshape(B, L, KV_HEADS, HEAD_DIM)
    y_l = window_sink_attention(q, k, v, kc, vc, sinks) @ w_out
    y_c = None
    if need_ctx:
        qc = (xc @ w_in[:, :Q_W]).reshape(B, L, ATT_HEADS, HEAD_DIM)
        y_c = ctx_sink_attention(qc, kc, vc, sinks) @ w_out
    return y_l, y_c


def ec_moe(h, router_w, w_gate, w_up, w_down):
    B, T, D = h.shape
    cap = EC_CAPACITY * T // N_EXPERTS
    aff = jax.nn.softmax((h @ router_w).astype(jnp.float32), axis=-1)
    gates, idx = lax.top_k(jnp.swapaxes(aff, 1, 2), cap)
    xg = jax.vmap(lambda hb, ib: hb[ib])(h, idx)
    hid = jax.nn.silu(jnp.einsum('becd,edf->becf', xg, w_gate)) * jnp.einsum('becd,edf->becf', xg, w_up)
    y = jnp.einsum('becf,efd->becd', hid, w_down) * gates[..., None].astype(h.dtype)
    return jax.vmap(lambda yb, ib: jnp.zeros((T, D), h.dtype).at[ib.reshape(-1)].add(yb.reshape(-1, D)))(y, idx)


def setup_inputs(seed: int = 0) -> dict:
    key = jax.random.key(seed)
    ks = iter(jax.random.split(key, 25))
    n_even = (DEPTH + 1) // 2
    n_odd = DEPTH // 2

    def normal(shape, s=1.0):
        return jax.random.normal(next(ks), shape, jnp.float32) * s

    def dense(shape, fan_in, gain=1.0):
        return normal(shape, gain * fan_in ** -0.5)

    def gain(shape):
        return 1.0 + normal(shape, 0.1)

    return {
        'x': normal((BATCH, SEQ, D_MODEL)),
        'c': normal((BATCH, D_MODEL)),
        'ctx': normal((BATCH, CTX_LEN, D_MODEL)),
        'c_ctx': normal((D_MODEL,)),
        'mod_w': dense((DEPTH, D_MODEL, N_MOD * D_MODEL), D_MODEL, 0.5),
        'mod_b': normal((DEPTH, N_MOD * D_MODEL), 0.02),
        'norm_mix': gain((DEPTH, D_MODEL)),
        'norm_ffn': gain((DEPTH, D_MODEL)),
        'ab_w_in': dense((n_even, D_MODEL, AB_IN_W), D_MODEL),
        'gla_gate_w': dense((n_even, 2, GLA_RANK, GLA_K_W), GLA_RANK),
        'gla_gate_b': normal((n_even, 2, GLA_K_W), 0.1),
        'gla_norm': gain((n_even, GLA_V_W)),
        'sg_norm_w': gain((n_even, SG_W)),
        'sg_norm_b': normal((n_even, SG_W), 0.02),
        'sg_ws': dense((n_even, SG_GROUPS, SG_CHUNK, SG_CHUNK), SG_CHUNK),
        'sg_bs': gain((n_even, SG_GROUPS, SG_CHUNK)),
        'ab_w_out': dense((n_even, MIX_W, D_MODEL), MIX_W),
        'c_w_in': dense((n_odd, D_MODEL, C_IN_W), D_MODEL),
        'sinks': normal((n_odd, ATT_HEADS), 0.5),
        'c_w_out': dense((n_odd, Q_W, D_MODEL), Q_W),
        'router_w': dense((DEPTH, D_MODEL, N_EXPERTS), D_MODEL),
        'moe_w_gate': dense((DEPTH, N_EXPERTS, D_MODEL, EXPERT_FF), D_MODEL),
        'moe_w_up': dense((DEPTH, N_EXPERTS, D_MODEL, EXPERT_FF), D_MODEL),
        'moe_w_down': dense((DEPTH, N_EXPERTS, EXPERT_FF, D_MODEL), EXPERT_FF),
        'final_norm': gain((D_MODEL,)),
    }


def reference(x, c, ctx, c_ctx, mod_w, mod_b, norm_mix, norm_ffn, ab_w_in, gla_gate_w, gla_gate_b, gla_norm,
              sg_norm_w, sg_norm_b, sg_ws, sg_bs, ab_w_out, c_w_in, sinks, c_w_out, router_w, moe_w_gate,
              moe_w_up, moe_w_down, final_norm):
    B, T, D = x.shape
    cos, sin = axial_rope_tables(T, x.dtype)
    h, hc = x, ctx
    for layer in range(DEPTH):
        last = layer == DEPTH - 1
        m_l = (jax.nn.silu(c) @ mod_w[layer] + mod_b[layer]).reshape(B, N_MOD, 1, D)
        m_l = [m_l[:, i] for i in range(N_MOD)]
        m_c = (jax.nn.silu(c_ctx) @ mod_w[layer] + mod_b[layer]).reshape(N_MOD, 1, 1, D)
        xl = modulate(h, norm_mix[layer], m_l[0], m_l[1])
        xc = modulate(hc, norm_mix[layer], m_c[0], m_c[1])
        if layer % 2 == 0:
            e = layer // 2
            y_l, y_c = ab_mixer(xl, xc, ab_w_in[e], gla_gate_w[e], gla_gate_b[e], gla_norm[e], sg_norm_w[e],
                                sg_norm_b[e], sg_ws[e], sg_bs[e], ab_w_out[e])
        else:
            o = layer // 2
            y_l, y_c = c_mixer(xl, xc, c_w_in[o], sinks[o], c_w_out[o], cos, sin, not last)
        moe = (router_w[layer], moe_w_gate[layer], moe_w_up[layer], moe_w_down[layer])
        h = h + m_l[2] * y_l
        h = h + m_l[5] * ec_moe(modulate(h, norm_ffn[layer], m_l[3], m_l[4]), *moe)
        if not last:
            hc = hc + m_c[2] * y_c
            hc = hc + m_c[5] * ec_moe(modulate(hc, norm_ffn[layer], m_c[3], m_c[4]), *moe)
    return rms_norm(h, final_norm)
```

```python
import numpy as np
from contextlib import ExitStack, contextmanager
import concourse.bass as bass
import concourse.mybir as mybir
from concourse.bass_utils import run_bass_kernel_spmd

F32 = mybir.dt.float32
BF16 = mybir.dt.bfloat16
I32 = mybir.dt.int32
U32 = mybir.dt.uint32
AF = mybir.ActivationFunctionType
ALU = mybir.AluOpType
AX = mybir.AxisListType

D = 4096
T = 4096
L = 256
NT = 34
NTOK = NT * 128
ABW = 10272
EPS = 1e-6
NEXP = 16
FF = 1024
CAP = 512
CAPC = 32


class Buf:
    __slots__ = ("name", "w", "r")

    def __init__(self, name=""):
        self.name = name
        self.w = []
        self.r = []


class TT:
    __slots__ = ("t", "b")

    def __init__(self, t, name=""):
        self.t = t
        self.b = Buf(name)


class Sched:
    NDMASEM = 32
    ROLL = 30000

    def __init__(self, nc, es):
        self.nc = nc
        self.es = es
        self.eng = {"pe": nc.tensor, "act": nc.scalar, "dve": nc.vector, "pool": nc.gpsimd, "sp": nc.sync}
        self.nsem = 0
        self.sem = {k: self._newsem() for k in self.eng}
        self.cnt = {k: 0 for k in self.eng}
        self.last = {k: None for k in self.eng}
        self.seen = {k: {} for k in self.eng}
        self.dsem = [self._newsem() for i in range(self.NDMASEM)]
        self.dcnt = [0] * self.NDMASEM
        self.dnext = 0
        self.ninst = 0

    def _newsem(self):
        self.nsem += 1
        return self.es.enter_context(self.nc.semaphore("sm%d" % self.nsem))

    def _wait(self, e, tok, relax=False):
        if tok is None:
            return
        sem, val, src = tok
        if src == "pe" and e == "pe":
            return
        if relax and src == e and e in ("act", "dve"):
            return
        key = id(sem)
        if self.seen[e].get(key, 0) >= val:
            return
        self.seen[e][key] = val
        self.eng[e].wait_ge(sem, val)

    def _deps(self, e, reads, writes, nowaw=False):
        for b in reads:
            for t in b.w:
                self._wait(e, t)
        for b in writes:
            if not nowaw:
                for t in b.w:
                    self._wait(e, t, relax=True)
            for t in b.r:
                self._wait(e, t, relax=True)

    def _done(self, tok, reads, writes, nowaw=False):
        for b in reads:
            b.r.append(tok)
            if len(b.r) > 48:
                b.r = b.r[-48:]
        for b in writes:
            if nowaw:
                b.w.append(tok)
            else:
                b.w = [tok]
            b.r = []

    def op(self, e, fn, reads=(), writes=()):
        reads = [x.b if isinstance(x, TT) else x for x in reads]
        writes = [x.b if isinstance(x, TT) else x for x in writes]
        self._deps(e, reads, writes)
        if self.cnt[e] >= self.ROLL:
            self.sem[e] = self._newsem()
            self.cnt[e] = 0
        inst = fn(self.eng[e])
        self.cnt[e] += 1
        inst.then_inc(self.sem[e], 1)
        tok = (self.sem[e], self.cnt[e], e)
        self.last[e] = tok
        self._done(tok, reads, writes)
        self.ninst += 1
        return tok

    def pe(self, fn, r=(), w=()):
        return self.op("pe", fn, r, w)

    def act(self, fn, r=(), w=()):
        return self.op("act", fn, r, w)

    def dve(self, fn, r=(), w=()):
        return self.op("dve", fn, r, w)

    def pool(self, fn, r=(), w=()):
        return self.op("pool", fn, r, w)

    def _dma_tok(self, q, inst):
        j = self.dnext
        self.dnext = (self.dnext + 1) % self.NDMASEM
        self.dcnt[j] += 16
        inst.then_inc(self.dsem[j], 16)
        return (self.dsem[j], self.dcnt[j], None)

    def _dma_pre(self, q):
        j = self.dnext
        if self.dcnt[j] > 0:
            self._wait(q, (self.dsem[j], self.dcnt[j], None))

    def dma(self, q, out, in_, reads=(), writes=(), nowaw=False, **kw):
        reads = [x.b if isinstance(x, TT) else x for x in reads]
        writes = [x.b if isinstance(x, TT) else x for x in writes]
        self._deps(q, reads, writes, nowaw)
        self._dma_pre(q)
        inst = self.eng[q].dma_start(out=out, in_=in_, **kw)
        tok = self._dma_tok(q, inst)
        self._done(tok, reads, writes, nowaw)
        self.ninst += 1
        return tok

    def idma(self, out, out_off, in_, in_off, reads=(), writes=(), nowaw=False, **kw):
        q = "pool"
        reads = [x.b if isinstance(x, TT) else x for x in reads]
        writes = [x.b if isinstance(x, TT) else x for x in writes]
        self._deps(q, reads, writes, nowaw)
        self._dma_pre(q)
        inst = self.nc.gpsimd.indirect_dma_start(out=out, out_offset=out_off, in_=in_, in_offset=in_off, **kw)
        tok = self._dma_tok(q, inst)
        self._done(tok, reads, writes, nowaw)
        self.ninst += 1
        return tok

    def barrier(self):
        toks = [t for t in self.last.values() if t is not None]
        toks += [(self.dsem[j], self.dcnt[j], None) for j in range(self.NDMASEM) if self.dcnt[j] > 0]
        for e in self.eng:
            for t in toks:
                if t[2] == e:
                    continue
                self._wait(e, t)


class KB:
    def __init__(self, nc, es, S):
        self.nc, self.es, self.S = nc, es, S
        self.st = None
        self.uid = 0

    @contextmanager
    def stage(self):
        self.S.barrier()
        with ExitStack() as st:
            old = self.st
            self.st = st
            yield st
            self.S.barrier()
            self.st = old

    def sb(self, shape, dt, name="t"):
        self.uid += 1
        nm = "%s_%d" % (name, self.uid)
        return TT((self.st or self.es).enter_context(self.nc.sbuf_tensor(nm, list(shape), dt)), nm)


def _cast_eng(i):
    return "dve" if i % 2 == 0 else "pool"


def _cast(S, eng, out_ap, in_ap, src, dst):
    if eng == "act":
        S.act(lambda e: e.copy(out=out_ap, in_=in_ap), [src], [dst])
    else:
        S.op(eng, lambda e: e.tensor_copy(out=out_ap, in_=in_ap), [src], [dst])


def linear_tok(K, xT, tiles, W, Kd, blocks, epi, wbufs, cast_engs=("dve", "pool")):
    S = K.S
    kc = Kd // 128
    npieces = max(1, kc // 8)
    cpp = kc // npieces
    wst, wbf = wbufs
    nbuf = 2 if len(tiles) <= 4 else 1
    pi = K.lin_pi
    for bi, (n0, n) in enumerate(blocks):
        for kp in range(npieces):
            ws, wb = wst[pi % len(wst)], wbf[pi % len(wbf)]
            src = W[kp * cpp * 128:(kp + 1) * cpp * 128, n0:n0 + n].rearrange("(c p) n -> p c n", p=128)
            S.dma("sp", ws.t[:, 0:cpp, 0:n], src, writes=[ws])
            _cast(S, cast_engs[pi % len(cast_engs)], wb.t[:, 0:cpp, 0:n], ws.t[:, 0:cpp, 0:n], ws, wb)
            for c in range(cpp):
                for ti, (c0, m) in enumerate(tiles):
                    ps = K.PS[ti + (4 * (bi % 2) if nbuf == 2 else 0)]
                    first = (kp == 0 and c == 0)
                    lastk = (kp == npieces - 1 and c == cpp - 1)
                    S.pe(lambda e: e.matmul(ps.t[0:m, 0:n], lhsT=xT.t[:, kp * cpp + c, c0:c0 + m], rhs=wb.t[:, c, 0:n],
                                            start=first, stop=lastk), [xT, wb], [ps])
            pi += 1
        for ti, (c0, m) in enumerate(tiles):
            ps = K.PS[ti + (4 * (bi % 2) if nbuf == 2 else 0)]
            epi(ti, bi, n0, n, ps)
    K.lin_pi = pi


def load_fm(K, vec_ap, out_ap, wr):
    S = K.S
    tmp = K.fm_tmp
    S.dma("sp", tmp.t[:, :], vec_ap.rearrange("(c p) -> c p", p=128), writes=[tmp])
    ps = K.PS[7]
    S.pe(lambda e: e.matmul(ps.t[:, 0:32], lhsT=tmp.t[0:32, :], rhs=K.ident.t[0:32, 0:32], start=True, stop=True),
         [tmp, K.ident], [ps])
    S.dve(lambda e: e.tensor_copy(out=out_ap, in_=ps.t[:, 0:32]), [ps], [wr])


def mod_tables(K, l, row, i_shift, i_scale, gain_ap, name):
    S = K.S
    g = K.sb([128, 32], F32, name + "g")
    sc = K.sb([128, 32], F32, name + "s")
    G1 = K.sb([128, 32], F32, name + "G1")
    SH = K.sb([128, 32], F32, name + "SH")
    load_fm(K, gain_ap, g.t[:, :], g)
    load_fm(K, K.MV[l, row, i_scale * D:(i_scale + 1) * D], sc.t[:, :], sc)
    load_fm(K, K.MV[l, row, i_shift * D:(i_shift + 1) * D], SH.t[:, :], SH)
    S.dve(lambda e: e.scalar_tensor_tensor(out=G1.t[:, :], in0=sc.t[:, :], scalar=1.0, in1=g.t[:, :], op0=ALU.add, op1=ALU.mult),
          [sc, g], [G1])
    return G1, SH


def rstd_of(K, x, n, ss, rs, junk, inv_n):
    S = K.S
    S.dve(lambda e: e.memset(ss.t[:, 0:1], 0.0), [], [ss])
    S.act(lambda e: e.activation(out=junk.t[:, 0:n], in_=x.t[:, 0:n], func=AF.Square, accum_out=ss.t[:, 0:1]), [x, ss], [junk, ss])
    S.act(lambda e: e.activation(out=rs.t[:, 0:1], in_=ss.t[:, 0:1], func=AF.Sqrt, scale=inv_n, bias=EPS), [ss], [rs])
    S.dve(lambda e: e.reciprocal(out=rs.t[:, 0:1], in_=rs.t[:, 0:1]), [rs], [rs])


def norm_T(K, x, G1, SH, xT, col0, bufs):
    S = K.S
    ss, rs, junk, dg = bufs
    rstd_of(K, x, D, ss, rs, junk, 1.0 / D)
    S.dve(lambda e: e.tensor_scalar_mul(out=dg.t[:, :], in0=K.ident.t[:, :], scalar1=rs.t[:, 0:1]), [K.ident, rs], [dg])
    for c in range(32):
        ps = K.PS[6 + (c // 4) % 2]
        S.pe(lambda e: e.matmul(ps.t[:, (c % 4) * 128:(c % 4 + 1) * 128], lhsT=x.t[:, c * 128:(c + 1) * 128], rhs=dg.t[:, :],
                                start=True, stop=True), [x, dg], [ps])
        S.act(lambda e: e.activation(out=xT.t[:, c, col0:col0 + 128], in_=ps.t[:, (c % 4) * 128:(c % 4 + 1) * 128],
                                     func=AF.Identity, scale=G1.t[:, c:c + 1], bias=SH.t[:, c:c + 1]), [ps, G1, SH], [xT])


def transpose_blocks(K, src, nblk, dst, col0, m=128, eng="act", src_off=0, dst_off=0, ident=None):
    S = K.S
    ident = ident or K.ident
    for c0 in range(0, nblk, 4):
        nb = min(4, nblk - c0)
        ps = K.PS[6 + (c0 // 4) % 2]
        for j in range(nb):
            c = c0 + j
            S.pe(lambda e: e.matmul(ps.t[:, j * 128:j * 128 + m], lhsT=src.t[0:m, src_off + c * 128:src_off + (c + 1) * 128],
                                    rhs=ident.t[0:m, 0:m], start=True, stop=True), [src, ident], [ps])
        pv = ps.t[:, 0:nb * 128].rearrange("p (j t) -> p j t", t=128)[:, :, 0:m]
        if eng == "act":
            S.act(lambda e: e.copy(out=dst.t[:, dst_off + c0:dst_off + c0 + nb, col0:col0 + m], in_=pv), [ps], [dst])
        else:
            S.op(eng, lambda e: e.tensor_copy(out=dst.t[:, dst_off + c0:dst_off + c0 + nb, col0:col0 + m], in_=pv), [ps], [dst])


def stage_mod(K):
    S = K.S
    with K.stage():
        sc = K.sb([128, 2, 32], F32, "sc")
        S.dma("sp", sc.t[:, 0, :], K.c_in.rearrange("(p k) -> p k", k=32), writes=[sc])
        S.dma("sp", sc.t[:, 1, :], K.c_ctx.rearrange("(p k) -> p k", k=32), writes=[sc])
        S.act(lambda e: e.activation(out=sc.t[:, :, :], in_=sc.t[:, :, :], func=AF.Silu), [sc], [sc])
        wst = [K.sb([128, 8, 512], F32, "mw") for _ in range(3)]
        mb = [K.sb([2, 512], F32, "mb") for _ in range(2)]
        mo = [K.sb([2, 512], F32, "mo") for _ in range(2)]
        pi = 0
        for l in range(2):
            Wv = K.mod_w[l].rearrange("(p k) n -> p k n", k=32)
            for nb in range(48):
                ps = K.PS[nb % 2]
                for kp in range(4):
                    ws = wst[pi % 3]
                    S.dma("sp", ws.t[:, :, :], Wv[:, kp * 8:(kp + 1) * 8, nb * 512:(nb + 1) * 512], writes=[ws])
                    for c in range(8):
                        S.pe(lambda e: e.matmul(ps.t[0:2, :], lhsT=sc.t[:, :, kp * 8 + c], rhs=ws.t[:, c, :],
                                                start=(kp == 0 and c == 0), stop=(kp == 3 and c == 7)), [sc, ws], [ps])
                    pi += 1
                b = mb[nb % 2]
                o = mo[nb % 2]
                S.dma("act", b.t[:, :], K.mod_b[l:l + 1, nb * 512:(nb + 1) * 512].partition_broadcast(2), writes=[b])
                S.dve(lambda e: e.tensor_tensor(out=o.t[:, :], in0=ps.t[0:2, :], in1=b.t[:, :], op=ALU.add), [ps, b], [o])
                S.dma("act", K.MV[l, :, nb * 512:(nb + 1) * 512], o.t[:, :], reads=[o])


GROUPS = [[0, 1]] + [[2 + 4 * g + i for i in range(4)] for g in range(8)]
GROUPS8 = [[0, 1]] + [[2 + 8 * g + i for i in range(8)] for g in range(4)]


def lin_bufs(K, nws=2, nwb=2):
    wst = [K.sb([128, 8, 512], F32, "wst") for _ in range(nws)]
    wbf = [K.sb([128, 8, 512], BF16, "wbf") for _ in range(nwb)]
    return wst, wbf


def norm_bufs(K):
    return (K.sb([128, 1], F32, "ss"), K.sb([128, 1], F32, "rs"), K.sb([128, D], BF16, "junk"), K.sb([128, 128], F32, "dg"))


def stage_inproj(K, l, W, ncols, OUT, gain_ap, ctx_cols=None):
    S = K.S
    with K.stage():
        G1l, SHl = mod_tables(K, l, 0, 0, 1, gain_ap, "l")
        G1c, SHc = mod_tables(K, l, 1, 0, 1, gain_ap, "c")
        xT = K.sb([128, 32, 1024], BF16, "xT")
        xt = [K.sb([128, D], F32, "xt") for _ in range(2)]
        nb_ = norm_bufs(K)
        wb_ = lin_bufs(K, 4, 2)
        ev = [K.sb([128, 512], F32, "ev") for _ in range(3)]
        evi = [0]
        blocks_all = [(n0, min(512, ncols - n0)) for n0 in range(0, ncols, 512)]
        xi = 0
        for grp in GROUPS8:
            isctx = grp[0] < 2
            for ti, t in enumerate(grp):
                x = xt[xi % 2]
                xi += 1
                S.dma("sp", x.t[:, :], K.H[t * 128:(t + 1) * 128, :], writes=[x])
                norm_T(K, x, G1c if isctx else G1l, SHc if isctx else SHl, xT, ti * 128, nb_)
            blocks = blocks_all
            if isctx and ctx_cols is not None:
                blocks = [b for b in blocks_all if b[0] >= ctx_cols[0] and b[0] < ctx_cols[1]]

            def epi(ti, bi, n0, n, ps, grp=grp):
                e_ = ev[evi[0] % 3]
                evi[0] += 1
                S.act(lambda e: e.copy(out=e_.t[:, 0:n], in_=ps.t[:, 0:n]), [ps], [e_])
                t = grp[ti]
                S.dma("act", OUT[t * 128:(t + 1) * 128, n0:n0 + n], e_.t[:, 0:n], reads=[e_])

            linear_tok(K, xT, [(i * 128, 128) for i in range(len(grp))], W, D, blocks, epi, wb_, cast_engs=("dve", "act"))


def stage_gla(K):
    S = K.S
    nc = K.nc
    with K.stage():
        cst = {}
        for i, nm in enumerate(["A1f", "A3f", "A1b", "A3b", "MKf", "MKb"]):
            cst[nm] = K.sb([128, 128], F32, nm)
            S.dma("sp", cst[nm].t[:, :], K.cmat[i], writes=[cst[nm]])
        CH = K.sb([128, 2], F32, "CH")
        S.dma("sp", CH.t[:, :], K.cch[:, :], writes=[CH])
        GW = []
        for d in range(2):
            g = K.sb([33, 1024], F32, "GW")
            S.dve(lambda e: e.memset(g.t[:, :], 0.0), [], [g])
            S.dma("sp", g.t[16 * d:16 * d + 16, :], K.gla_gate_w[d], writes=[g])
            S.dma("sp", g.t[32:33, :], K.gla_gate_b[d:d + 1, :], writes=[g])
            GW.append(g)
        St32 = [[K.sb([128, 2, 512], F32, "S32") for h in range(4)] for d in range(2)]
        St16 = [[K.sb([128, 2, 512], BF16, "S16") for h in range(4)] for d in range(2)]
        for d in range(2):
            for h in range(4):
                S.dve(lambda e: e.memset(St32[d][h].t[:, :, :], 0.0), [], [St32[d][h]])
                S.pool(lambda e: e.memset(St16[d][h].t[:, :, :], 0.0), [], [St16[d][h]])
        B = []
        for d in range(2):
            b = dict(
                q32=K.sb([128, 1024], F32, "q32"), k32=K.sb([128, 1024], F32, "k32"), v32=K.sb([128, 2048], F32, "v32"),
                lr=K.sb([128, 32], F32, "lr"), v16=K.sb([128, 2048], BF16, "v16"), lrT=K.sb([33, 128], F32, "lrT"),
                l32=K.sb([128, 1024], F32, "l32"), e1=K.sb([128, 1024], F32, "e1"), e2=K.sb([128, 1024], F32, "e2"),
                e3=K.sb([128, 1024], F32, "e3"), kdec=K.sb([128, 1024], BF16, "kdec"), dec=K.sb([128, 16], F32, "dec"),
                qinT=K.sb([128, 8, 128], BF16, "qinT"), kinT=K.sb([128, 8, 128], BF16, "kinT"),
                sT=K.sb([128, 128], BF16, "sT"), o32=K.sb([128, 2048], F32, "o32"))
            S.dve(lambda e: e.memset(b["lrT"].t[:, :], 1.0), [], [b["lrT"]])
            B.append(b)
        PS = K.PS
        order = [[0, 1] + list(range(2, 34)), [1, 0] + list(range(33, 1, -1))]
        for step in range(NT):
            for d in range(2):
                t = order[d][step]
                b = B[d]
                r0 = t * 128
                A1 = cst["A1f"] if d == 0 else cst["A1b"]
                A3 = cst["A3f"] if d == 0 else cst["A3b"]
                MK = cst["MKf"] if d == 0 else cst["MKb"]
                q32, k32, v32, lr, v16, lrT, l32 = b["q32"], b["k32"], b["v32"], b["lr"], b["v16"], b["lrT"], b["l32"]
                e1, e2, e3, kdec, dec, qinT, kinT, sT, o32 = b["e1"], b["e2"], b["e3"], b["kdec"], b["dec"], b["qinT"], b["kinT"], b["sT"], b["o32"]
                S.dma("sp", q32.t[:, :], K.P0[r0:r0 + 128, 0:1024], writes=[q32])
                S.dma("sp", k32.t[:, :], K.P0[r0:r0 + 128, 1024:2048], writes=[k32])
                S.dma("sp", v32.t[:, :], K.P0[r0:r0 + 128, 2048:4096], writes=[v32])
                S.dma("sp", lr.t[:, :], K.P0[r0:r0 + 128, 6144:6176], writes=[lr])
                S.pool(lambda e: e.tensor_copy(out=v16.t[:, :], in_=v32.t[:, :]), [v32], [v16])
                S.pe(lambda e: e.matmul(PS[4].t[0:32, 128:256], lhsT=lr.t[:, 0:32], rhs=K.ident.t[:, :], start=True, stop=True),
                     [lr, K.ident], [PS[4]])
                S.dve(lambda e: e.tensor_copy(out=lrT.t[0:32, :], in_=PS[4].t[0:32, 128:256]), [PS[4]], [lrT])
                for blk in range(2):
                    S.pe(lambda e: e.matmul(PS[blk].t[:, :], lhsT=lrT.t[0:33, :], rhs=GW[d].t[0:33, blk * 512:(blk + 1) * 512],
                                            start=True, stop=True), [lrT, GW[d]], [PS[blk]])
                    S.act(lambda e: e.activation(out=l32.t[:, blk * 512:(blk + 1) * 512], in_=PS[blk].t[:, :], func=AF.Exp, scale=-1.0),
                          [PS[blk]], [l32])
                S.act(lambda e: e.activation(out=l32.t[:, :], in_=l32.t[:, :], func=AF.Ln, bias=1.0), [l32], [l32])
                for blk in range(2):
                    S.pe(lambda e: e.matmul(PS[blk].t[:, :], lhsT=A1.t[:, :], rhs=l32.t[:, blk * 512:(blk + 1) * 512], start=True, stop=True),
                         [A1, l32], [PS[blk]])
                    S.pe(lambda e: e.matmul(PS[2 + blk].t[:, :], lhsT=A3.t[:, :], rhs=l32.t[:, blk * 512:(blk + 1) * 512], start=True, stop=True),
                         [A3, l32], [PS[2 + blk]])
                    sl = slice(blk * 512, (blk + 1) * 512)
                    S.act(lambda e: e.activation(out=e1.t[:, sl], in_=PS[blk].t[:, :], func=AF.Exp), [PS[blk]], [e1])
                    S.act(lambda e: e.activation(out=e2.t[:, sl], in_=PS[blk].t[:, :], func=AF.Exp, scale=-1.0), [PS[blk]], [e2])
                    S.act(lambda e: e.activation(out=e3.t[:, sl], in_=PS[2 + blk].t[:, :], func=AF.Exp), [PS[2 + blk]], [e3])
                S.dve(lambda e: e.scalar_tensor_tensor(out=e1.t[:, :], in0=q32.t[:, :], scalar=1.0 / 16.0, in1=e1.t[:, :],
                                                       op0=ALU.mult, op1=ALU.mult), [q32, e1], [e1])
                S.dve(lambda e: e.tensor_tensor(out=e2.t[:, :], in0=k32.t[:, :], in1=e2.t[:, :], op=ALU.mult), [k32, e2], [e2])
                S.pool(lambda e: e.tensor_tensor(out=kdec.t[:, :], in0=k32.t[:, :], in1=e3.t[:, :], op=ALU.mult), [k32, e3], [kdec])
                for db in range(8):
                    S.pe(lambda e: e.matmul(PS[4].t[:, db * 2:db * 2 + 2], lhsT=l32.t[:, db * 128:(db + 1) * 128], rhs=CH.t[:, 0:2],
                                            start=True, stop=True), [l32, CH], [PS[4]])
                S.act(lambda e: e.activation(out=dec.t[:, 0:16], in_=PS[4].t[:, 0:16], func=AF.Exp), [PS[4]], [dec])
                transpose_blocks(K, e1, 8, qinT, 0, eng="act")
                transpose_blocks(K, e2, 8, kinT, 0, eng="dve")
                for h in range(4):
                    s32, s16 = St32[d][h], St16[d][h]
                    for m in range(2):
                        S.pe(lambda e: e.matmul(PS[5].t[:, 0:128], lhsT=kinT.t[:, 2 * h + m, :], rhs=qinT.t[:, 2 * h + m, :],
                                                start=(m == 0), stop=(m == 1)), [kinT, qinT], [PS[5]])
                    S.dve(lambda e: e.tensor_tensor(out=sT.t[:, :], in0=PS[5].t[:, 0:128], in1=MK.t[:, :], op=ALU.mult), [PS[5], MK], [sT])
                    pso = PS[6 + h % 2]
                    S.pe(lambda e: e.matmul(pso.t[:, :], lhsT=sT.t[:, :], rhs=v16.t[:, h * 512:(h + 1) * 512], start=True, stop=False),
                         [sT, v16], [pso])
                    chunks = [0, 1] if d == 0 else [1, 0]
                    for idx, ci in enumerate(chunks):
                        rows = slice(ci * 64, ci * 64 + 64)
                        for m in range(2):
                            S.pe(lambda e: e.matmul(pso.t[rows, :], lhsT=qinT.t[:, 2 * h + m, rows], rhs=s16.t[:, m, :],
                                                    start=False, stop=(idx == 1 and m == 1)), [qinT, s16], [pso])
                        for m in range(2):
                            psu = PS[2 + m]
                            S.pe(lambda e: e.matmul(psu.t[:, :], lhsT=kdec.t[rows, (2 * h + m) * 128:(2 * h + m + 1) * 128],
                                                    rhs=v16.t[rows, h * 512:(h + 1) * 512], start=True, stop=True), [kdec, v16], [psu])
                            dcol = (2 * h + m) * 2 + ci
                            S.dve(lambda e: e.scalar_tensor_tensor(out=s32.t[:, m, :], in0=s32.t[:, m, :], scalar=dec.t[:, dcol:dcol + 1],
                                                                   in1=psu.t[:, :], op0=ALU.mult, op1=ALU.add), [s32, dec, psu], [s32])
                            S.act(lambda e: e.copy(out=s16.t[:, m, :], in_=s32.t[:, m, :]), [s32], [s16])
                    S.act(lambda e: e.copy(out=o32.t[:, h * 512:(h + 1) * 512], in_=pso.t[:, :]), [pso], [o32])
                S.dma("act", K.OG[d, r0:r0 + 128, :], o32.t[:, :], reads=[o32])


def load_bc(K, vec_ap, n, name):
    t = K.sb([128, n], F32, name)
    K.S.dma("sp", t.t[:, :], vec_ap.partition_broadcast(128), writes=[t])
    return t


def stage_merge(K, l):
    S = K.S
    PS = K.PS
    with K.stage():
        gnbc = load_bc(K, K.gla_norm[0:1, :], 2048, "gnbc")
        nwbc = load_bc(K, K.sg_norm_w[0:1, :], 2048, "nwbc")
        nbbc = load_bc(K, K.sg_norm_b[0:1, :], 2048, "nbbc")
        wsT = K.sb([128, 4, 128], BF16, "wsT")
        wtmp = K.sb([128, 512], F32, "wtmp")
        S.dma("sp", wtmp.t[:, :].rearrange("p (g j) -> p g j", g=4), K.sg_ws.rearrange("g i j -> i g j"), writes=[wtmp])
        transpose_blocks(K, wtmp, 4, wsT, 0, eng="dve")
        bsT = K.sb([128, 4], F32, "bsT")
        btmp = K.sb([4, 128], F32, "btmp")
        S.dma("sp", btmp.t[:, :], K.sg_bs[:, :], writes=[btmp])
        S.pe(lambda e: e.matmul(PS[7].t[:, 0:4], lhsT=btmp.t[0:4, :], rhs=K.ident.t[0:4, 0:4], start=True, stop=True), [btmp, K.ident], [PS[7]])
        S.dve(lambda e: e.tensor_copy(out=bsT.t[:, :], in_=PS[7].t[:, 0:4]), [PS[7]], [bsT])
        sets = []
        for _ in range(2):
            sets.append(dict(of=K.sb([128, 2048], F32, "of"), ob=K.sb([128, 2048], F32, "ob"), g32=K.sb([128, 2048], F32, "g32"),
                             u32=K.sb([128, 2048], F32, "u32"), vs32=K.sb([128, 2048], F32, "vs32"),
                             mTn=K.sb([128, 32, 128], BF16, "mTn"), st4=[K.sb([128, 4], F32, "st4") for _ in range(4)]))
        mrg = K.sb([128, D], F32, "mrg")
        vn16 = K.sb([128, 2048], BF16, "vn16")
        junk = K.sb([128, 512], BF16, "junk")
        XTv = K.XT.rearrange("(c p) t -> p c t", p=128)
        for grp in [list(range(NT))]:
            for ti, t in enumerate(grp):
                r0 = t * 128
                st_ = sets[t % 2]
                of, ob, g32, u32, vs32, mTn, st4 = st_["of"], st_["ob"], st_["g32"], st_["u32"], st_["vs32"], st_["mTn"], st_["st4"]
                S.dma("sp", of.t[:, :], K.OG[0, r0:r0 + 128, :], writes=[of])
                S.dma("sp", ob.t[:, :], K.OG[1, r0:r0 + 128, :], writes=[ob])
                S.dma("sp", g32.t[:, :], K.P0[r0:r0 + 128, 4096:6144], writes=[g32])
                S.dma("sp", u32.t[:, :], K.P0[r0:r0 + 128, 6176:8224], writes=[u32])
                S.dma("sp", vs32.t[:, :], K.P0[r0:r0 + 128, 8224:10272], writes=[vs32])
                ss, rs, mu, var = st4
                S.dve(lambda e: e.tensor_tensor(out=of.t[:, :], in0=of.t[:, :], in1=ob.t[:, :], op=ALU.add), [of, ob], [of])
                S.dve(lambda e: e.memset(ss.t[:, :], 0.0), [], [ss])
                for h in range(4):
                    S.act(lambda e: e.activation(out=junk.t[:, :], in_=of.t[:, h * 512:(h + 1) * 512], func=AF.Square,
                                                 accum_out=ss.t[:, h:h + 1]), [of, ss], [junk, ss])
                S.act(lambda e: e.activation(out=rs.t[:, :], in_=ss.t[:, :], func=AF.Sqrt, scale=1.0 / 512, bias=EPS), [ss], [rs])
                S.dve(lambda e: e.reciprocal(out=rs.t[:, :], in_=rs.t[:, :]), [rs], [rs])
                for h in range(4):
                    sl = slice(h * 512, (h + 1) * 512)
                    S.dve(lambda e: e.scalar_tensor_tensor(out=of.t[:, sl], in0=of.t[:, sl], scalar=rs.t[:, h:h + 1], in1=gnbc.t[:, sl],
                                                           op0=ALU.mult, op1=ALU.mult), [of, rs, gnbc], [of])
                S.act(lambda e: e.activation(out=g32.t[:, :], in_=g32.t[:, :], func=AF.Silu), [g32], [g32])
                S.dve(lambda e: e.tensor_tensor(out=mrg.t[:, 0:2048], in0=of.t[:, :], in1=g32.t[:, :], op=ALU.mult), [of, g32], [mrg])
                S.act(lambda e: e.activation(out=u32.t[:, :], in_=u32.t[:, :], func=AF.Gelu_apprx_tanh), [u32], [u32])
                S.act(lambda e: e.activation(out=vs32.t[:, :], in_=vs32.t[:, :], func=AF.Gelu_apprx_tanh), [vs32], [vs32])
                S.dve(lambda e: e.tensor_reduce(out=mu.t[:, :], in_=vs32.t[:, :].rearrange("p (g c) -> p g c", g=4), axis=AX.X, op=ALU.add),
                      [vs32], [mu])
                S.dve(lambda e: e.tensor_scalar_mul(out=mu.t[:, :], in0=mu.t[:, :], scalar1=-1.0 / 512), [mu], [mu])
                S.dve(lambda e: e.memset(var.t[:, :], 0.0), [], [var])
                for g in range(4):
                    sl = slice(g * 512, (g + 1) * 512)
                    S.act(lambda e: e.activation(out=vs32.t[:, sl], in_=vs32.t[:, sl], func=AF.Identity, bias=mu.t[:, g:g + 1], scale=1.0),
                          [vs32, mu], [vs32])
                    S.act(lambda e: e.activation(out=junk.t[:, :], in_=vs32.t[:, sl], func=AF.Square, accum_out=var.t[:, g:g + 1]),
                          [vs32, var], [junk, var])
                S.act(lambda e: e.activation(out=var.t[:, :], in_=var.t[:, :], func=AF.Sqrt, scale=1.0 / 512, bias=EPS), [var], [var])
                S.dve(lambda e: e.reciprocal(out=var.t[:, :], in_=var.t[:, :]), [var], [var])
                for g in range(4):
                    sl = slice(g * 512, (g + 1) * 512)
                    S.dve(lambda e: e.scalar_tensor_tensor(out=vs32.t[:, sl], in0=vs32.t[:, sl], scalar=var.t[:, g:g + 1], in1=nwbc.t[:, sl],
                                                           op0=ALU.mult, op1=ALU.mult), [vs32, var, nwbc], [vs32])
                    S.pool(lambda e: e.tensor_tensor(out=vn16.t[:, sl], in0=vs32.t[:, sl], in1=nbbc.t[:, sl], op=ALU.add), [vs32, nbbc], [vn16])
                    ps = PS[4 + g % 2]
                    S.pe(lambda e: e.matmul(ps.t[:, :], lhsT=wsT.t[:, g, :], rhs=vn16.t[:, sl], start=True, stop=True), [wsT, vn16], [ps])
                    S.dve(lambda e: e.scalar_tensor_tensor(out=mrg.t[:, 2048 + g * 512:2048 + (g + 1) * 512], in0=ps.t[:, :],
                                                           scalar=bsT.t[:, g:g + 1], in1=u32.t[:, sl], op0=ALU.add, op1=ALU.mult),
                          [ps, bsT, u32], [mrg])
                transpose_blocks(K, mrg, 32, mTn, 0, eng="act")
                S.dma("act", XTv[:, :, r0:r0 + 128], mTn.t[:, :, :], reads=[mTn])
    stage_outproj(K, l, K.ab_w_out, list(range(NT)))


def stage_outproj(K, l, W, tiles_all):
    S = K.S
    with K.stage():
        m2 = [load_bc(K, K.MV[l, r:r + 1, 2 * D:3 * D], D, "m2") for r in range(2)]
        xT = K.sb([128, 32, 1024], BF16, "xT")
        wb_ = lin_bufs(K, 4, 2)
        hb = [K.sb([128, 512], F32, "hb") for _ in range(3)]
        hi = [0]
        XTv = K.XT.rearrange("(c p) t -> p c t", p=128)
        groups = []
        if tiles_all[0] < 2:
            groups.append([0, 1])
        lat = [t for t in tiles_all if t >= 2]
        for i in range(0, len(lat), 8):
            groups.append(lat[i:i + 8])
        for grp in groups:
            isctx = grp[0] < 2
            n = len(grp) * 128
            S.dma("sp", xT.t[:, :, 0:n], XTv[:, :, grp[0] * 128:grp[0] * 128 + n], writes=[xT])
            mm = m2[1] if isctx else m2[0]

            def epi(ti, bi, n0, nn, ps, grp=grp, mm=mm):
                h_ = hb[hi[0] % 3]
                hi[0] += 1
                t = grp[ti]
                S.dma("sp", h_.t[:, 0:nn], K.H[t * 128:(t + 1) * 128, n0:n0 + nn], writes=[h_])
                S.dve(lambda e: e.tensor_tensor(out=ps.t[:, 0:nn], in0=ps.t[:, 0:nn], in1=mm.t[:, n0:n0 + nn], op=ALU.mult), [ps, mm], [ps])
                S.dve(lambda e: e.tensor_tensor(out=h_.t[:, 0:nn], in0=h_.t[:, 0:nn], in1=ps.t[:, 0:nn], op=ALU.add), [h_, ps], [h_])
                S.dma("act", K.H[t * 128:(t + 1) * 128, n0:n0 + nn], h_.t[:, 0:nn], reads=[h_])

            linear_tok(K, xT, [(i * 128, 128) for i in range(len(grp))], W, D, [(n0, 512) for n0 in range(0, D, 512)], epi, wb_, cast_engs=("act", "pool"))


def stage_moe(K, l, with_ctx):
    S = K.S
    PS = K.PS
    rows = [1, 0] if with_ctx else [0]
    with K.stage():
        IDXT = K.sb([128, 4, 16], I32, "IDXT")
        GATE = K.sb([128, 4, 16], F32, "GATE")
        IDXTC = K.sb([32, 16], I32, "IDXTC")
        GATEC = K.sb([32, 16], F32, "GATEC")
        IDX8 = K.sb([128, 4, 16], I32, "IDX8")
        IDX8C = K.sb([32, 16], I32, "IDX8C")
        afs = ExitStack()
        old_st = K.st
        K.st = afs
        AFFT = K.sb([16, NTOK], F32, "AFFT")
        K.st = old_st
        with K.stage():
            gbc = load_bc(K, K.norm_ffn[l:l + 1, :], D, "gbc")
            G1 = K.sb([128, D], F32, "G1bc")
            SH = K.sb([128, D], F32, "SHbc")
            RW = K.sb([128, 32, 16], F32, "RW")
            S.dma("sp", RW.t[:, :, :], K.router_w[l].rearrange("(c p) e -> p c e", p=128), writes=[RW])
            xt = [K.sb([128, D], F32, "xt") for _ in range(2)]
            hm = K.sb([128, D], F32, "hm")
            hmT = K.sb([128, 32, 128], F32, "hmT")
            hm16 = K.sb([128, D], BF16, "hm16")
            ss, rs, junk, dg = norm_bufs(K)
            sm = [K.sb([128, 16], F32, "sm") for _ in range(6)]
            xi = 0
            for r in rows:
                S.dma("sp", G1.t[:, :], K.MV[l, r:r + 1, 4 * D:5 * D].partition_broadcast(128), writes=[G1])
                S.dma("sp", SH.t[:, :], K.MV[l, r:r + 1, 3 * D:4 * D].partition_broadcast(128), writes=[SH])
                S.dve(lambda e: e.scalar_tensor_tensor(out=G1.t[:, :], in0=G1.t[:, :], scalar=1.0, in1=gbc.t[:, :], op0=ALU.add, op1=ALU.mult),
                      [G1, gbc], [G1])
                for t in ([0, 1] if r == 1 else range(2, NT)):
                    x = xt[xi % 2]
                    xi += 1
                    S.dma("sp", x.t[:, :], K.H[t * 128:(t + 1) * 128, :], writes=[x])
                    rstd_of(K, x, D, ss, rs, junk, 1.0 / D)
                    S.dve(lambda e: e.scalar_tensor_tensor(out=hm.t[:, :], in0=x.t[:, :], scalar=rs.t[:, 0:1], in1=G1.t[:, :],
                                                           op0=ALU.mult, op1=ALU.mult), [x, rs, G1], [hm])
                    S.pool(lambda e: e.tensor_tensor(out=hm.t[:, :], in0=hm.t[:, :], in1=SH.t[:, :], op=ALU.add), [hm, SH], [hm])
                    S.act(lambda e: e.copy(out=hm16.t[:, :], in_=hm.t[:, :]), [hm], [hm16])
                    S.dma("act", K.HM[t * 128:(t + 1) * 128, :], hm16.t[:, :], reads=[hm16])
                    transpose_blocks(K, hm, 32, hmT, 0, eng="act")
                    for c in range(32):
                        S.pe(lambda e: e.matmul(PS[5].t[:, 0:16], lhsT=hmT.t[:, c, :], rhs=RW.t[:, c, :], start=(c == 0), stop=(c == 31)),
                             [hmT, RW], [PS[5]])
                    mx, nmx, sme, ex, rsm, aff = sm
                    S.dve(lambda e: e.tensor_reduce(out=mx.t[:, 0:1], in_=PS[5].t[:, 0:16], axis=AX.X, op=ALU.max), [PS[5]], [mx])
                    S.dve(lambda e: e.tensor_scalar_mul(out=nmx.t[:, 0:1], in0=mx.t[:, 0:1], scalar1=-1.0), [mx], [nmx])
                    S.dve(lambda e: e.memset(sme.t[:, 0:1], 0.0), [], [sme])
                    S.act(lambda e: e.activation(out=ex.t[:, :], in_=PS[5].t[:, 0:16], func=AF.Exp, bias=nmx.t[:, 0:1], scale=1.0,
                                                 accum_out=sme.t[:, 0:1]), [PS[5], nmx, sme], [ex, sme])
                    S.dve(lambda e: e.reciprocal(out=rsm.t[:, 0:1], in_=sme.t[:, 0:1]), [sme], [rsm])
                    S.dve(lambda e: e.tensor_scalar_mul(out=aff.t[:, :], in0=ex.t[:, :], scalar1=rsm.t[:, 0:1]), [ex, rsm], [aff])
                    S.pe(lambda e: e.matmul(PS[4].t[0:16, 0:128], lhsT=aff.t[:, 0:16], rhs=K.ident.t[:, :], start=True, stop=True),
                         [aff, K.ident], [PS[4]])
                    S.dve(lambda e: e.tensor_copy(out=AFFT.t[0:16, t * 128:(t + 1) * 128], in_=PS[4].t[0:16, 0:128]), [PS[4]], [AFFT])
        with K.stage():
            Wk = K.sb([16, T], F32, "Wk")
            VALS = K.sb([16, CAP + CAPC], F32, "VALS")
            IDX = K.sb([16, CAP + CAPC], U32, "IDX")
            S.dve(lambda e: e.tensor_copy(out=Wk.t[:, :], in_=AFFT.t[:, L:NTOK]), [AFFT], [Wk])
            for r in range(CAP // 8):
                sl = slice(r * 8, r * 8 + 8)
                S.dve(lambda e: e.max(out=VALS.t[:, sl], in_=Wk.t[:, :]), [Wk], [VALS])
                S.dve(lambda e: e.max_index(out=IDX.t[:, sl], in_max=VALS.t[:, sl], in_values=Wk.t[:, :]), [Wk, VALS], [IDX])
                S.dve(lambda e: e.match_replace(out=Wk.t[:, :], in_to_replace=VALS.t[:, sl], in_values=Wk.t[:, :], imm_value=-1.0),
                      [Wk, VALS], [Wk])
            if with_ctx:
                S.dve(lambda e: e.tensor_copy(out=Wk.t[:, 0:L], in_=AFFT.t[:, 0:L]), [AFFT, Wk], [Wk])
                for r in range(CAPC // 8):
                    sl = slice(CAP + r * 8, CAP + r * 8 + 8)
                    S.dve(lambda e: e.max(out=VALS.t[:, sl], in_=Wk.t[:, 0:L]), [Wk], [VALS])
                    S.dve(lambda e: e.max_index(out=IDX.t[:, sl], in_max=VALS.t[:, sl], in_values=Wk.t[:, 0:L]), [Wk, VALS], [IDX])
                    S.dve(lambda e: e.match_replace(out=Wk.t[:, 0:L], in_to_replace=VALS.t[:, sl], in_values=Wk.t[:, 0:L], imm_value=-1.0),
                          [Wk, VALS], [Wk])
            else:
                S.dve(lambda e: e.memset(VALS.t[:, CAP:CAP + CAPC], 0.0), [], [VALS])
                S.dve(lambda e: e.memset(IDX.t[:, CAP:CAP + CAPC], 0), [], [IDX])
            bI = Buf("idxd")
            bV = Buf("vald")
            S.dma("sp", K.IDXD[l, :, :], IDX.t[:, :].bitcast(I32), reads=[IDX], writes=[bI])
            S.dma("sp", K.VALD[l, :, :], VALS.t[:, :], reads=[VALS], writes=[bV])
            for j in range(4):
                S.dma("sp", IDXT.t[:, j, :], K.IDXD[l, :, j * 128:(j + 1) * 128].rearrange("e p -> p e"), reads=[bI], writes=[IDXT],
                      allow_slow_non_contiguous=True)
                S.dma("sp", GATE.t[:, j, :], K.VALD[l, :, j * 128:(j + 1) * 128].rearrange("e p -> p e"), reads=[bV], writes=[GATE],
                      allow_slow_non_contiguous=True)
            S.dma("sp", IDXTC.t[:, :], K.IDXD[l, :, CAP:CAP + CAPC].rearrange("e p -> p e"), reads=[bI], writes=[IDXTC],
                  allow_slow_non_contiguous=True)
            S.dma("sp", GATEC.t[:, :], K.VALD[l, :, CAP:CAP + CAPC].rearrange("e p -> p e"), reads=[bV], writes=[GATEC],
                  allow_slow_non_contiguous=True)
            S.dve(lambda e: e.tensor_single_scalar(out=IDX8.t[:, :, :], in_=IDXT.t[:, :, :], scalar=3, op=ALU.logical_shift_left), [IDXT], [IDX8])
            S.dve(lambda e: e.tensor_single_scalar(out=IDX8C.t[:, :], in_=IDXTC.t[:, :], scalar=3, op=ALU.logical_shift_left), [IDXTC], [IDX8C])
            S.dma("sp", K.DBG8[l, :, :], IDX8.t[:, :, :].rearrange("p j e -> p (j e)"), reads=[IDX8])
        S.barrier()
        afs.close()
        with K.stage():
            m5 = [load_bc(K, K.MV[l, r:r + 1, 5 * D:6 * D], D, "m5") for r in ([0, 1] if with_ctx else [0])]
            ntile = 5 if with_ctx else 4
            xg = [K.sb([128, D], BF16, "xg") for _ in range(ntile)]
            ncol = CAP + (CAPC if with_ctx else 0)
            xgT = K.sb([128, 32, ncol], BF16, "xgT")
            gs = K.sb([128, 4, ncol], F32, "gs")
            hidT = K.sb([128, 8, ncol], BF16, "hidT")
            wst, wbf = lin_bufs(K, 3, 2)
            ysb = [K.sb([128, 512], F32, "ysb") for _ in range(3)]
            id16 = K.sb([128, 128], BF16, "id16")
            S.dve(lambda e: e.tensor_copy(out=id16.t[:, :], in_=K.ident.t[:, :]), [K.ident], [id16])
            yi = [0]
            HCB = [Buf("hcb%d" % i) for i in range(8)]
            H8 = K.H.rearrange("t (nb c) -> (t nb) c", c=512)
            ce = ("dve", "act")
            pi = 0

            def gather_expert(ex_):
                for j in range(4):
                    S.idma(xg[j].t[:, :], None, K.HM[:, :], bass.IndirectOffsetOnAxis(ap=IDXT.t[:, j, ex_:ex_ + 1], axis=0),
                           reads=[IDXT], writes=[xg[j]], element_offset=L * D)
                if with_ctx:
                    S.idma(xg[4].t[0:CAPC, :], None, K.HM[:, :], bass.IndirectOffsetOnAxis(ap=IDXTC.t[0:CAPC, ex_:ex_ + 1], axis=0),
                           reads=[IDXTC], writes=[xg[4]])

            def transposes_expert(ex_):
                for j in range(4):
                    transpose_blocks(K, xg[j], 32, xgT, j * 128, eng="act" if j % 2 == 0 else "dve", ident=id16)
                if with_ctx:
                    transpose_blocks(K, xg[4], 32, xgT, CAP, m=CAPC, eng="act", ident=id16)

            gather_expert(0)
            transposes_expert(0)
            for ex_ in range(NEXP):
                if ex_ + 1 < NEXP:
                    gather_expert(ex_ + 1)
                for half in range(2):
                    for wi, Wm in enumerate((K.moe_w_gate[l, ex_], K.moe_w_up[l, ex_])):
                        for kp in range(4):
                            ws, wb = wst[pi % 3], wbf[pi % 2]
                            src = Wm[kp * 1024:(kp + 1) * 1024, half * 512:(half + 1) * 512].rearrange("(c p) n -> p c n", p=128)
                            S.dma("sp", ws.t[:, :, :], src, writes=[ws])
                            _cast(S, ce[pi % 2], wb.t[:, :, :], ws.t[:, :, :], ws, wb)
                            pi += 1
                            for c in range(8):
                                first = (kp == 0 and c == 0)
                                lastk = (kp == 3 and c == 7)
                                for fb in range(4):
                                    S.pe(lambda e: e.matmul(PS[fb].t[:, 0:CAP], lhsT=wb.t[:, c, fb * 128:(fb + 1) * 128],
                                                            rhs=xgT.t[:, kp * 8 + c, 0:CAP], start=first, stop=lastk), [wb, xgT], [PS[fb]])
                                    if with_ctx:
                                        S.pe(lambda e: e.matmul(PS[4 + fb].t[:, 0:CAPC], lhsT=wb.t[:, c, fb * 128:(fb + 1) * 128],
                                                                rhs=xgT.t[:, kp * 8 + c, CAP:ncol], start=first, stop=lastk), [wb, xgT], [PS[4 + fb]])
                        for fb in range(4):
                            if wi == 0:
                                S.act(lambda e: e.activation(out=gs.t[:, fb, 0:CAP], in_=PS[fb].t[:, 0:CAP], func=AF.Silu), [PS[fb]], [gs])
                            else:
                                S.dve(lambda e: e.tensor_tensor(out=hidT.t[:, half * 4 + fb, 0:CAP], in0=gs.t[:, fb, 0:CAP], in1=PS[fb].t[:, 0:CAP],
                                                                op=ALU.mult), [gs, PS[fb]], [hidT])
                        if with_ctx:
                            for fb in range(4):
                                pv = PS[4 + fb].t[:, 0:CAPC]
                                if wi == 0:
                                    S.act(lambda e: e.activation(out=gs.t[:, fb, CAP:ncol], in_=pv, func=AF.Silu), [PS[4 + fb]], [gs])
                                else:
                                    S.dve(lambda e: e.tensor_tensor(out=hidT.t[:, half * 4 + fb, CAP:ncol], in0=gs.t[:, fb, CAP:ncol], in1=pv,
                                                                    op=ALU.mult), [gs, PS[4 + fb]], [hidT])
                if ex_ + 1 < NEXP:
                    transposes_expert(ex_ + 1)

                def epi(ti, bi, n0, n, ps, ex_=ex_):
                    y = ysb[yi[0] % 3]
                    yi[0] += 1
                    if ti < 4:
                        S.dve(lambda e: e.scalar_tensor_tensor(out=y.t[:, :], in0=ps.t[:, :], scalar=GATE.t[:, ti, ex_:ex_ + 1], in1=m5[0].t[:, n0:n0 + n],
                                                               op0=ALU.mult, op1=ALU.mult), [ps, GATE, m5[0]], [y])
                        S.idma(H8[:, :], bass.IndirectOffsetOnAxis(ap=IDX8.t[:, ti, ex_:ex_ + 1], axis=0), y.t[:, :], None,
                               reads=[y, IDX8], writes=[HCB[bi]], nowaw=(ti > 0), compute_op=ALU.add, element_offset=L * D + n0)
                    else:
                        S.dve(lambda e: e.scalar_tensor_tensor(out=y.t[0:CAPC, :], in0=ps.t[0:CAPC, :], scalar=GATEC.t[0:CAPC, ex_:ex_ + 1],
                                                               in1=m5[1].t[0:CAPC, n0:n0 + n], op0=ALU.mult, op1=ALU.mult), [ps, GATEC, m5[1]], [y])
                        S.idma(H8[:, :], bass.IndirectOffsetOnAxis(ap=IDX8C.t[0:CAPC, ex_:ex_ + 1], axis=0), y.t[0:CAPC, :], None,
                               reads=[y, IDX8C], writes=[HCB[bi]], nowaw=True, compute_op=ALU.add, element_offset=n0)

                tl = [(i * 128, 128) for i in range(4)] + ([(CAP, CAPC)] if with_ctx else [])
                K.lin_pi = pi
                linear_tok(K, hidT, tl, K.moe_w_down[l, ex_], FF, [(n0, 512) for n0 in range(0, D, 512)], epi, (wst, wbf), cast_engs=ce)
                pi = K.lin_pi


def rope(K, xs_ap, src, out, tmp, cos, sin):
    S = K.S
    S.dve(lambda e: e.tensor_tensor(out=tmp.t[:, :], in0=xs_ap, in1=cos.t[:, :], op=ALU.mult), [src, cos], [tmp])
    xv = xs_ap.rearrange("p (a b c) -> p a b c", b=2, c=32)
    sv = sin.t[:, :].rearrange("p (a b c) -> p a b c", b=2, c=32)
    ov = out.t[:, :].rearrange("p (a b c) -> p a b c", b=2, c=32)
    S.pool(lambda e: e.tensor_tensor(out=ov[:, :, 0, :], in0=xv[:, :, 1, :], in1=sv[:, :, 0, :], op=ALU.mult), [src, sin], [out])
    S.pool(lambda e: e.tensor_tensor(out=ov[:, :, 1, :], in0=xv[:, :, 0, :], in1=sv[:, :, 1, :], op=ALU.mult), [src, sin], [out])
    S.dve(lambda e: e.tensor_tensor(out=out.t[:, :], in0=out.t[:, :], in1=tmp.t[:, :], op=ALU.add), [out, tmp], [out])


def stage_attn(K, l=1):
    S = K.S
    PS = K.PS
    P1 = K.P1
    with K.stage():
        mk32 = K.sb([128, 512], F32, "mk32")
        MK = [K.sb([128, 512], BF16, "MK") for _ in range(2)]
        for i in range(2):
            S.dma("sp", mk32.t[:, :], K.amask[i], writes=[mk32])
            S.dve(lambda e: e.tensor_copy(out=MK[i].t[:, :], in_=mk32.t[:, :]), [mk32], [MK[i]])
        sk = K.sb([1, 32], F32, "sk")
        S.dma("sp", sk.t[:, :], K.sinks[0:1, :], writes=[sk])
        S.act(lambda e: e.activation(out=sk.t[:, :], in_=sk.t[:, :], func=AF.Exp), [sk], [sk])
        ones1 = K.sb([1, 128], F32, "ones1")
        S.dve(lambda e: e.memset(ones1.t[:, :], 1.0), [], [ones1])
        ESROW = K.sb([1, 32, 128], BF16, "ESROW")
        for h in range(32):
            S.dve(lambda e: e.tensor_scalar_mul(out=ESROW.t[0:1, h, :], in0=ones1.t[0:1, :], scalar1=sk.t[0:1, h:h + 1]), [ones1, sk], [ESROW])
        ONE1 = K.sb([1, 129], BF16, "ONE1")
        S.dve(lambda e: e.memset(ONE1.t[:, :], 0.0), [], [ONE1])
        S.dve(lambda e: e.memset(ONE1.t[:, 128:129], 1.0), [], [ONE1])
        KTC = K.sb([128, 8, 256], BF16, "KTC")
        VC = K.sb([128, 2, 8, 129], BF16, "VC")
        KT = [K.sb([128, 8, 128], BF16, "KT") for _ in range(4)]
        VA = [K.sb([128, 8, 129], BF16, "VA") for _ in range(4)]
        S.pool(lambda e: e.memset(VC.t[:, :, :, :], 1.0), [], [VC])
        for i in range(4):
            S.pool(lambda e: e.memset(VA[i].t[:, :, :], 1.0), [], [VA[i]])
        k32 = K.sb([128, 1024], F32, "k32")
        v32 = K.sb([128, 1024], F32, "v32")
        kr = K.sb([128, 1024], F32, "kr")
        tmp = K.sb([128, 1024], F32, "tmp")
        cosk = K.sb([128, 1024], F32, "cosk")
        sink = K.sb([128, 1024], F32, "sink")
        cosq = cosk
        sinq = sink
        q32 = K.sb([128, D], F32, "q32")
        QT = K.sb([128, 32, 128], BF16, "QT")
        p16 = [K.sb([128, 512], BF16, "p16") for _ in range(3)]
        aTn = [K.sb([128, 32, 128], BF16, "aTn") for _ in range(2)]
        XTv = K.XT.rearrange("(c p) t -> p c t", p=128)
        for tc in range(2):
            S.dma("sp", k32.t[:, :], P1[tc * 128:(tc + 1) * 128, 4096:5120], writes=[k32])
            transpose_blocks(K, k32, 8, KTC, tc * 128, eng="act")
            S.dma("sp", v32.t[:, :], P1[tc * 128:(tc + 1) * 128, 5120:6144], writes=[v32])
            S.pool(lambda e: e.tensor_copy(out=VC.t[:, tc, :, 0:128], in_=v32.t[:, :].rearrange("p (h d) -> p h d", d=128)), [v32], [VC])

        def prep_kv(t):
            slot = t % 4
            p0 = (t - 2) * 128
            S.dma("sp", k32.t[:, :], P1[t * 128:(t + 1) * 128, 4096:5120], writes=[k32])
            S.dma("sp", v32.t[:, :], P1[t * 128:(t + 1) * 128, 5120:6144], writes=[v32])
            S.dma("sp", cosk.t[:, :], K.cosk[p0:p0 + 128, :], writes=[cosk])
            S.dma("sp", sink.t[:, :], K.sink[p0:p0 + 128, :], writes=[sink])
            rope(K, k32.t[:, :], k32, kr, tmp, cosk, sink)
            transpose_blocks(K, kr, 8, KT[slot], 0, eng="act")
            S.pool(lambda e: e.tensor_copy(out=VA[slot].t[:, :, 0:128], in_=v32.t[:, :].rearrange("p (h d) -> p h d", d=128)), [v32], [VA[slot]])

        pcount = 0
        kc = 0
        ones16 = K.sb([128, 128], BF16, "ones16")
        S.dve(lambda e: e.memset(ones16.t[:, :], 1.0), [], [ones16])
        ones1b = K.sb([1, 128], BF16, "ones1b")
        S.dve(lambda e: e.memset(ones1b.t[:, :], 1.0), [], [ones1b])
        QT2 = [QT, K.sb([128, 32, 128], BF16, "QTb")]
        rec = [K.sb([128, 512], F32, "rec") for _ in range(2)]

        def qprep(n):
            t = n + 2
            p0 = n * 128
            S.dma("sp", q32.t[:, :], P1[t * 128:(t + 1) * 128, 0:4096], writes=[q32])
            S.dma("sp", cosq.t[:, :], K.cosk[p0:p0 + 128, :], writes=[cosq])
            S.dma("sp", sinq.t[:, :], K.sink[p0:p0 + 128, :], writes=[sinq])
            for grp in range(4):
                rope(K, q32.t[:, grp * 1024:(grp + 1) * 1024], q32, kr, tmp, cosq, sinq)
                transpose_blocks(K, kr, 8, QT2[n % 2], 0, eng="dve", dst_off=grp * 8)

        prep_kv(2)
        prep_kv(3)
        qprep(0)
        for n in range(32):
            t = n + 2
            qt = QT2[n % 2]
            aT = aTn[n % 2]
            for kvh in range(8):
                if kvh == 1 and t + 2 <= 33:
                    prep_kv(t + 2)
                if kvh == 4 and n + 1 < 32:
                    qprep(n + 1)
                blks = []
                if t - 1 >= 2:
                    blks.append((KT[(t - 1) % 4], slice(0, 128), VA[(t - 1) % 4], VA[(t - 1) % 4].t[:, kvh, 0:128], MK[0]))
                blks.append((KT[t % 4], slice(0, 128), VA[t % 4], VA[t % 4].t[:, kvh, 0:128], None))
                if t + 1 <= 33:
                    blks.append((KT[(t + 1) % 4], slice(0, 128), VA[(t + 1) % 4], VA[(t + 1) % 4].t[:, kvh, 0:128], MK[1]))
                for tc in range(2):
                    blks.append((KTC, slice(tc * 128, (tc + 1) * 128), VC, VC.t[:, tc, kvh, 0:128], None))
                po = PS[kc % 2]
                pd = PS[2 + kc % 2]
                for bi_, (kt, jc, vt, vap, mk) in enumerate(blks):
                    psx = PS[4 + bi_ % 2]
                    S.pe(lambda e: e.matmul(psx.t[:, :], lhsT=kt.t[:, kvh, jc], rhs=qt.t[:, kvh * 4:(kvh + 1) * 4, :].rearrange("p h i -> p (h i)"),
                                            start=True, stop=True), [kt, qt], [psx])
                    p = p16[pcount % 3]
                    pcount += 1
                    S.act(lambda e: e.activation(out=p.t[:, :], in_=psx.t[:, :], func=AF.Exp, scale=128.0 ** -0.5), [psx], [p])
                    if mk is not None:
                        S.pool(lambda e: e.tensor_tensor(out=p.t[:, :], in0=p.t[:, :], in1=mk.t[:, :], op=ALU.mult), [p, mk], [p])
                    S.pe(lambda e: e.matmul(po.t[:, :], lhsT=vap, rhs=p.t[:, :], start=(bi_ == 0), stop=(bi_ == len(blks) - 1)), [p, vt], [po])
                    S.pe(lambda e: e.matmul(pd.t[:, :], lhsT=ones16.t[:, :], rhs=p.t[:, :], start=(bi_ == 0), stop=False), [p, ones16], [pd])
                S.pe(lambda e: e.matmul(pd.t[:, :], lhsT=ones1b.t[0:1, :], rhs=ESROW.t[0:1, kvh * 4:(kvh + 1) * 4, :].rearrange("p h i -> p (h i)"),
                                        start=False, stop=True), [ESROW, ones1b], [pd])
                r_ = rec[kc % 2]
                kc += 1
                S.dve(lambda e: e.reciprocal(out=r_.t[:, :], in_=pd.t[:, :]), [pd], [r_])
                S.dve(lambda e: e.tensor_tensor(out=aT.t[:, kvh * 4:(kvh + 1) * 4, :],
                                                in0=po.t[:, :].rearrange("p (h i) -> p h i", i=128),
                                                in1=r_.t[:, :].rearrange("p (h i) -> p h i", i=128), op=ALU.mult), [po, r_], [aT])
            S.dma("act", XTv[:, :, t * 128:(t + 1) * 128], aT.t[:, :, :], reads=[aT])
    stage_outproj(K, l, K.c_w_out, list(range(2, NT)))


def stage_final(K):
    S = K.S
    with K.stage():
        fbc = load_bc(K, K.final_norm[0:1, :], D, "fbc")
        xt = [K.sb([128, D], F32, "xt") for _ in range(2)]
        yt = [K.sb([128, D], F32, "yt") for _ in range(2)]
        ss, rs, junk, dg = norm_bufs(K)
        toks = []
        for i, t in enumerate(range(2, NT)):
            x, y = xt[i % 2], yt[i % 2]
            S.dma("sp", x.t[:, :], K.H[t * 128:(t + 1) * 128, :], writes=[x])
            rstd_of(K, x, D, ss, rs, junk, 1.0 / D)
            S.dve(lambda e: e.scalar_tensor_tensor(out=y.t[:, :], in0=x.t[:, :], scalar=rs.t[:, 0:1], in1=fbc.t[:, :], op0=ALU.mult, op1=ALU.mult),
                  [x, rs, fbc], [y])
            toks.append(S.dma("act", K.out[(t - 2) * 128:(t - 1) * 128, :], y.t[:, :], reads=[y]))
        for tk in toks:
            S._wait("act", tk)
            S._wait("sp", tk)

def build(debug=None, upto=9):
    nc = bass.Bass("TRN2", target_bir_lowering=False)
    dt_in = lambda name, shape, dt=F32: nc.dram_tensor(name, list(shape), dt, kind="ExternalInput").ap()
    dbg = debug or ()
    scr = lambda name, shape, dt=F32: nc.dram_tensor(name, list(shape), dt, kind=("ExternalOutput" if name in dbg else "Internal")).ap()
    with ExitStack() as es:
        S = Sched(nc, es)
        K = KB(nc, es, S)
        K.x_in = dt_in("x", [T, D])
        K.ctx_in = dt_in("ctx", [L, D])
        K.c_in = dt_in("c", [D])
        K.c_ctx = dt_in("c_ctx", [D])
        K.mod_w = dt_in("mod_w", [2, D, 6 * D])
        K.mod_b = dt_in("mod_b", [2, 6 * D])
        K.norm_mix = dt_in("norm_mix", [2, D])
        K.norm_ffn = dt_in("norm_ffn", [2, D])
        K.ab_w_in = dt_in("ab_w_in", [D, ABW])
        K.gla_gate_w = dt_in("gla_gate_w", [2, 16, 1024])
        K.gla_gate_b = dt_in("gla_gate_b", [2, 1024])
        K.gla_norm = dt_in("gla_norm", [1, 2048])
        K.sg_norm_w = dt_in("sg_norm_w", [1, 2048])
        K.sg_norm_b = dt_in("sg_norm_b", [1, 2048])
        K.sg_ws = dt_in("sg_ws", [4, 128, 128])
        K.sg_bs = dt_in("sg_bs", [4, 128])
        K.ab_w_out = dt_in("ab_w_out", [D, D])
        K.router_w = dt_in("router_w", [2, D, NEXP])
        K.moe_w_gate = dt_in("moe_w_gate", [2, NEXP, D, FF])
        K.moe_w_up = dt_in("moe_w_up", [2, NEXP, D, FF])
        K.moe_w_down = dt_in("moe_w_down", [2, NEXP, FF, D])
        K.final_norm = dt_in("final_norm", [1, D])
        K.c_w_in = dt_in("c_w_in", [D, 6144])
        K.c_w_out = dt_in("c_w_out", [D, D])
        K.sinks = dt_in("sinks", [1, 32])
        K.cosk = dt_in("cosk", [T, 1024])
        K.sink = dt_in("sink", [T, 1024])
        K.amask = dt_in("amask", [2, 128, 512])
        K.cmat = dt_in("cmat", [6, 128, 128])
        K.cch = dt_in("cch", [128, 2])
        K.ident_in = dt_in("ident", [128, 128])
        K.out = nc.dram_tensor("out", [T, D], F32, kind="ExternalOutput").ap()
        K.MV = scr("MV", [2, 2, 6 * D])
        K.H = scr("H", [NTOK, D])
        K.P0 = scr("P0", [NTOK, ABW])
        K.OG = scr("OG", [2, NTOK, 2048])
        K.HM = scr("HM", [NTOK, D], BF16)
        K.P1 = scr("P1", [NTOK, 6144])
        K.XT = scr("XT", [D, NTOK], BF16)
        K.dbg_attn = "AO" in dbg
        if K.dbg_attn:
            K.AO = scr("AO", [T, D])
            K.RQ = scr("RQ", [T, D])
            K.RK = scr("RK", [T, 1024])
        K.IDXD = scr("IDXD", [2, NEXP, CAP + CAPC], I32)
        K.VALD = scr("VALD", [2, NEXP, CAP + CAPC])
        K.DBG8 = scr("DBG8", [2, 128, 64], I32)
        K.PS = [TT(es.enter_context(nc.psum_tensor("ps%d" % i, [128, 512], F32)), "ps%d" % i) for i in range(8)]
        K.ident = TT(es.enter_context(nc.sbuf_tensor("ident_sb", [128, 128], F32)), "ident")
        K.fm_tmp = TT(es.enter_context(nc.sbuf_tensor("fm_tmp", [32, 128], F32)), "fm_tmp")
        K.lin_pi = 0
        S.dma("sp", K.ident.t[:, :], K.ident_in[:, :], writes=[K.ident])
        S.dma("sp", K.H[0:L, :], K.ctx_in[:, :])
        for i in range(4):
            S.dma("sp", K.H[L + i * 1024:L + (i + 1) * 1024, :], K.x_in[i * 1024:(i + 1) * 1024, :])
        stage_mod(K)
        stage_inproj(K, 0, K.ab_w_in, ABW, K.P0, K.norm_mix[0])
        stage_gla(K)
        stage_merge(K, 0)
        if upto >= 1:
            stage_moe(K, 0, True)
        if upto >= 2:
            stage_inproj(K, 1, K.c_w_in, 6144, K.P1, K.norm_mix[1], ctx_cols=(4096, 6144))
            stage_attn(K, 1)
        if upto >= 3:
            stage_moe(K, 1, False)
        stage_final(K)
        print("instructions:", S.ninst, "sems:", S.nsem)
    return nc


def host_consts():
    j = np.arange(128)[:, None]
    i = np.arange(128)[None, :]
    same = (j // 64) == (i // 64)
    a = -1.0 / 16.0
    A1f = np.where(same & (j <= i), a, 0.0)
    A3f = np.where(same & (j > i), a, 0.0)
    A1b = np.where(same & (j >= i), a, 0.0)
    A3b = np.where(same & (j < i), a, 0.0)
    MKf = np.where(same & (j <= i), 1.0, 0.0)
    MKb = np.where(same & (j >= i), 1.0, 0.0)
    cmat = np.stack([A1f, A3f, A1b, A3b, MKf, MKb]).astype(np.float32)
    cch = np.zeros((128, 2), np.float32)
    cch[:64, 0] = a
    cch[64:, 1] = a
    amask = np.stack([np.tile((j >= i), (1, 4)), np.tile((j <= i), (1, 4))]).astype(np.float32)
    pos = np.arange(T)
    row, col = pos // 64, pos % 64
    inv_freq = (10000.0 ** (-np.arange(0, 64, 2, dtype=np.float32) / 64.0)).astype(np.float32)
    def ang(p):
        a = p.astype(np.float32)[:, None] * inv_freq
        return np.concatenate([a, a], axis=-1)
    an = np.concatenate([ang(row), ang(col)], axis=-1).astype(np.float32)
    cos = np.cos(an).astype(np.float32)
    sin = np.sin(an).astype(np.float32)
    sgn = np.tile(np.concatenate([-np.ones(32), np.ones(32)]), 2).astype(np.float32)
    cosk = np.ascontiguousarray(np.tile(cos, (1, 8)))
    sink = np.ascontiguousarray(np.tile(sin * sgn, (1, 8)))
    return dict(cmat=cmat, cch=cch, ident=np.eye(128, dtype=np.float32), amask=amask, cosk=cosk, sink=sink)


def make_in_map(inp, b):
    f = lambda a: np.ascontiguousarray(a, dtype=np.float32)
    m = dict(
        x=f(inp["x"][b]), ctx=f(inp["ctx"][b]), c=f(inp["c"][b]), c_ctx=f(inp["c_ctx"]),
        mod_w=f(inp["mod_w"]), mod_b=f(inp["mod_b"]), norm_mix=f(inp["norm_mix"]), norm_ffn=f(inp["norm_ffn"]),
        ab_w_in=f(inp["ab_w_in"][0]), gla_gate_w=f(inp["gla_gate_w"][0]), gla_gate_b=f(inp["gla_gate_b"][0]),
        gla_norm=f(inp["gla_norm"]), sg_norm_w=f(inp["sg_norm_w"]), sg_norm_b=f(inp["sg_norm_b"]),
        sg_ws=f(inp["sg_ws"][0]), sg_bs=f(inp["sg_bs"][0]), ab_w_out=f(inp["ab_w_out"][0]),
        router_w=f(inp["router_w"]), moe_w_gate=f(inp["moe_w_gate"]), moe_w_up=f(inp["moe_w_up"]), moe_w_down=f(inp["moe_w_down"]),
        final_norm=f(inp["final_norm"]).reshape(1, D),
        c_w_in=f(inp["c_w_in"][0]), c_w_out=f(inp["c_w_out"][0]), sinks=f(inp["sinks"]).reshape(1, 32),
    )
    m.update(host_consts())
    return m


def kernel(**inputs):
    nc = build()
    in_maps = [make_in_map(inputs, b) for b in range(4)]
    res = run_bass_kernel_spmd(nc, in_maps, core_ids=list(range(4)))
    return np.stack([res.results[b]["out"] for b in range(4)], axis=0)
```

```python
import numpy as np
from contextlib import ExitStack, contextmanager
import concourse.bass as bass
import concourse.mybir as mybir
from concourse.bass_utils import run_bass_kernel_spmd

F32 = mybir.dt.float32
BF16 = mybir.dt.bfloat16
I32 = mybir.dt.int32
U32 = mybir.dt.uint32
AF = mybir.ActivationFunctionType
ALU = mybir.AluOpType
AX = mybir.AxisListType

D = 4096
T = 4096
L = 256
NT = 34
NTOK = NT * 128
ABW = 10272
EPS = 1e-6
NEXP = 16
FF = 1024
CAP = 512
CAPC = 32


class Buf:
    __slots__ = ("name", "w", "r")

    def __init__(self, name=""):
        self.name = name
        self.w = []
        self.r = []


class TT:
    __slots__ = ("t", "b")

    def __init__(self, t, name=""):
        self.t = t
        self.b = Buf(name)


class Sched:
    NDMASEM = 32
    ROLL = 30000

    def __init__(self, nc, es):
        self.nc = nc
        self.es = es
        self.eng = {"pe": nc.tensor, "act": nc.scalar, "dve": nc.vector, "pool": nc.gpsimd, "sp": nc.sync}
        self.nsem = 0
        self.sem = {k: self._newsem() for k in self.eng}
        self.cnt = {k: 0 for k in self.eng}
        self.last = {k: None for k in self.eng}
        self.seen = {k: {} for k in self.eng}
        self.dsem = [self._newsem() for i in range(self.NDMASEM)]
        self.dcnt = [0] * self.NDMASEM
        self.dnext = 0
        self.ninst = 0

    def _newsem(self):
        self.nsem += 1
        return self.es.enter_context(self.nc.semaphore("sm%d" % self.nsem))

    def _wait(self, e, tok, relax=False):
        if tok is None:
            return
        sem, val, src = tok
        if src == "pe" and e == "pe":
            return
        if relax and src == e and e in ("act", "dve"):
            return
        key = id(sem)
        if self.seen[e].get(key, 0) >= val:
            return
        self.seen[e][key] = val
        self.eng[e].wait_ge(sem, val)

    def _deps(self, e, reads, writes, nowaw=False):
        for b in reads:
            for t in b.w:
                self._wait(e, t)
        for b in writes:
            if not nowaw:
                for t in b.w:
                    self._wait(e, t, relax=True)
            for t in b.r:
                self._wait(e, t, relax=True)

    def _done(self, tok, reads, writes, nowaw=False):
        for b in reads:
            b.r.append(tok)
            if len(b.r) > 48:
                b.r = b.r[-48:]
        for b in writes:
            if nowaw:
                b.w.append(tok)
            else:
                b.w = [tok]
            b.r = []

    def op(self, e, fn, reads=(), writes=()):
        reads = [x.b if isinstance(x, TT) else x for x in reads]
        writes = [x.b if isinstance(x, TT) else x for x in writes]
        self._deps(e, reads, writes)
        if self.cnt[e] >= self.ROLL:
            self.sem[e] = self._newsem()
            self.cnt[e] = 0
        inst = fn(self.eng[e])
        self.cnt[e] += 1
        inst.then_inc(self.sem[e], 1)
        tok = (self.sem[e], self.cnt[e], e)
        self.last[e] = tok
        self._done(tok, reads, writes)
        self.ninst += 1
        return tok

    def pe(self, fn, r=(), w=()):
        return self.op("pe", fn, r, w)

    def act(self, fn, r=(), w=()):
        return self.op("act", fn, r, w)

    def dve(self, fn, r=(), w=()):
        return self.op("dve", fn, r, w)

    def pool(self, fn, r=(), w=()):
        return self.op("pool", fn, r, w)

    def _dma_tok(self, q, inst):
        j = self.dnext
        self.dnext = (self.dnext + 1) % self.NDMASEM
        self.dcnt[j] += 16
        inst.then_inc(self.dsem[j], 16)
        return (self.dsem[j], self.dcnt[j], None)

    def _dma_pre(self, q):
        j = self.dnext
        if self.dcnt[j] > 0:
            self._wait(q, (self.dsem[j], self.dcnt[j], None))

    def dma(self, q, out, in_, reads=(), writes=(), nowaw=False, **kw):
        reads = [x.b if isinstance(x, TT) else x for x in reads]
        writes = [x.b if isinstance(x, TT) else x for x in writes]
        self._deps(q, reads, writes, nowaw)
        self._dma_pre(q)
        inst = self.eng[q].dma_start(out=out, in_=in_, **kw)
        tok = self._dma_tok(q, inst)
        self._done(tok, reads, writes, nowaw)
        self.ninst += 1
        return tok

    def idma(self, out, out_off, in_, in_off, reads=(), writes=(), nowaw=False, **kw):
        q = "pool"
        reads = [x.b if isinstance(x, TT) else x for x in reads]
        writes = [x.b if isinstance(x, TT) else x for x in writes]
        self._deps(q, reads, writes, nowaw)
        self._dma_pre(q)
        inst = self.nc.gpsimd.indirect_dma_start(out=out, out_offset=out_off, in_=in_, in_offset=in_off, **kw)
        tok = self._dma_tok(q, inst)
        self._done(tok, reads, writes, nowaw)
        self.ninst += 1
        return tok

    def barrier(self):
        toks = [t for t in self.last.values() if t is not None]
        toks += [(self.dsem[j], self.dcnt[j], None) for j in range(self.NDMASEM) if self.dcnt[j] > 0]
        for e in self.eng:
            for t in toks:
                if t[2] == e:
                    continue
                self._wait(e, t)


class KB:
    def __init__(self, nc, es, S):
        self.nc, self.es, self.S = nc, es, S
        self.st = None
        self.uid = 0

    @contextmanager
    def stage(self):
        self.S.barrier()
        with ExitStack() as st:
            old = self.st
            self.st = st
            yield st
            self.S.barrier()
            self.st = old

    def sb(self, shape, dt, name="t"):
        self.uid += 1
        nm = "%s_%d" % (name, self.uid)
        return TT((self.st or self.es).enter_context(self.nc.sbuf_tensor(nm, list(shape), dt)), nm)


def _cast_eng(i):
    return "dve" if i % 2 == 0 else "pool"


def _cast(S, eng, out_ap, in_ap, src, dst):
    if eng == "act":
        S.act(lambda e: e.copy(out=out_ap, in_=in_ap), [src], [dst])
    else:
        S.op(eng, lambda e: e.tensor_copy(out=out_ap, in_=in_ap), [src], [dst])


def linear_tok(K, xT, tiles, W, Kd, blocks, epi, wbufs, cast_engs=("dve", "pool")):
    S = K.S
    kc = Kd // 128
    npieces = max(1, kc // 8)
    cpp = kc // npieces
    wst, wbf = wbufs
    nbuf = 2 if len(tiles) <= 4 else 1
    pi = K.lin_pi
    for bi, (n0, n) in enumerate(blocks):
        for kp in range(npieces):
            ws, wb = wst[pi % len(wst)], wbf[pi % len(wbf)]
            src = W[kp * cpp * 128:(kp + 1) * cpp * 128, n0:n0 + n].rearrange("(c p) n -> p c n", p=128)
            S.dma("sp", ws.t[:, 0:cpp, 0:n], src, writes=[ws])
            _cast(S, cast_engs[pi % len(cast_engs)], wb.t[:, 0:cpp, 0:n], ws.t[:, 0:cpp, 0:n], ws, wb)
            for c in range(cpp):
                for ti, (c0, m) in enumerate(tiles):
                    ps = K.PS[ti + (4 * (bi % 2) if nbuf == 2 else 0)]
                    first = (kp == 0 and c == 0)
                    lastk = (kp == npieces - 1 and c == cpp - 1)
                    S.pe(lambda e: e.matmul(ps.t[0:m, 0:n], lhsT=xT.t[:, kp * cpp + c, c0:c0 + m], rhs=wb.t[:, c, 0:n],
                                            start=first, stop=lastk), [xT, wb], [ps])
            pi += 1
        for ti, (c0, m) in enumerate(tiles):
            ps = K.PS[ti + (4 * (bi % 2) if nbuf == 2 else 0)]
            epi(ti, bi, n0, n, ps)
    K.lin_pi = pi


def load_fm(K, vec_ap, out_ap, wr):
    S = K.S
    tmp = K.fm_tmp
    S.dma("sp", tmp.t[:, :], vec_ap.rearrange("(c p) -> c p", p=128), writes=[tmp])
    ps = K.PS[7]
    S.pe(lambda e: e.matmul(ps.t[:, 0:32], lhsT=tmp.t[0:32, :], rhs=K.ident.t[0:32, 0:32], start=True, stop=True),
         [tmp, K.ident], [ps])
    S.dve(lambda e: e.tensor_copy(out=out_ap, in_=ps.t[:, 0:32]), [ps], [wr])


def mod_tables(K, l, row, i_shift, i_scale, gain_ap, name):
    S = K.S
    g = K.sb([128, 32], F32, name + "g")
    sc = K.sb([128, 32], F32, name + "s")
    G1 = K.sb([128, 32], F32, name + "G1")
    SH = K.sb([128, 32], F32, name + "SH")
    load_fm(K, gain_ap, g.t[:, :], g)
    load_fm(K, K.MV[l, row, i_scale * D:(i_scale + 1) * D], sc.t[:, :], sc)
    load_fm(K, K.MV[l, row, i_shift * D:(i_shift + 1) * D], SH.t[:, :], SH)
    S.dve(lambda e: e.scalar_tensor_tensor(out=G1.t[:, :], in0=sc.t[:, :], scalar=1.0, in1=g.t[:, :], op0=ALU.add, op1=ALU.mult),
          [sc, g], [G1])
    return G1, SH


def rstd_of(K, x, n, ss, rs, junk, inv_n):
    S = K.S
    S.dve(lambda e: e.memset(ss.t[:, 0:1], 0.0), [], [ss])
    S.act(lambda e: e.activation(out=junk.t[:, 0:n], in_=x.t[:, 0:n], func=AF.Square, accum_out=ss.t[:, 0:1]), [x, ss], [junk, ss])
    S.act(lambda e: e.activation(out=rs.t[:, 0:1], in_=ss.t[:, 0:1], func=AF.Sqrt, scale=inv_n, bias=EPS), [ss], [rs])
    S.dve(lambda e: e.reciprocal(out=rs.t[:, 0:1], in_=rs.t[:, 0:1]), [rs], [rs])


def norm_T(K, x, G1, SH, xT, col0, bufs):
    S = K.S
    ss, rs, junk, dg = bufs
    rstd_of(K, x, D, ss, rs, junk, 1.0 / D)
    S.dve(lambda e: e.tensor_scalar_mul(out=dg.t[:, :], in0=K.ident.t[:, :], scalar1=rs.t[:, 0:1]), [K.ident, rs], [dg])
    for c in range(32):
        ps = K.PS[6 + (c // 4) % 2]
        S.pe(lambda e: e.matmul(ps.t[:, (c % 4) * 128:(c % 4 + 1) * 128], lhsT=x.t[:, c * 128:(c + 1) * 128], rhs=dg.t[:, :],
                                start=True, stop=True), [x, dg], [ps])
        S.act(lambda e: e.activation(out=xT.t[:, c, col0:col0 + 128], in_=ps.t[:, (c % 4) * 128:(c % 4 + 1) * 128],
                                     func=AF.Identity, scale=G1.t[:, c:c + 1], bias=SH.t[:, c:c + 1]), [ps, G1, SH], [xT])


def transpose_blocks(K, src, nblk, dst, col0, m=128, eng="act", src_off=0, dst_off=0, ident=None):
    S = K.S
    ident = ident or K.ident
    for c0 in range(0, nblk, 4):
        nb = min(4, nblk - c0)
        ps = K.PS[6 + (c0 // 4) % 2]
        for j in range(nb):
            c = c0 + j
            S.pe(lambda e: e.matmul(ps.t[:, j * 128:j * 128 + m], lhsT=src.t[0:m, src_off + c * 128:src_off + (c + 1) * 128],
                                    rhs=ident.t[0:m, 0:m], start=True, stop=True), [src, ident], [ps])
        pv = ps.t[:, 0:nb * 128].rearrange("p (j t) -> p j t", t=128)[:, :, 0:m]
        if eng == "act":
            S.act(lambda e: e.copy(out=dst.t[:, dst_off + c0:dst_off + c0 + nb, col0:col0 + m], in_=pv), [ps], [dst])
        else:
            S.op(eng, lambda e: e.tensor_copy(out=dst.t[:, dst_off + c0:dst_off + c0 + nb, col0:col0 + m], in_=pv), [ps], [dst])


def stage_mod(K):
    S = K.S
    with K.stage():
        sc = K.sb([128, 2, 32], F32, "sc")
        S.dma("sp", sc.t[:, 0, :], K.c_in.rearrange("(p k) -> p k", k=32), writes=[sc])
        S.dma("sp", sc.t[:, 1, :], K.c_ctx.rearrange("(p k) -> p k", k=32), writes=[sc])
        S.act(lambda e: e.activation(out=sc.t[:, :, :], in_=sc.t[:, :, :], func=AF.Silu), [sc], [sc])
        wst = [K.sb([128, 8, 512], F32, "mw") for _ in range(3)]
        mb = [K.sb([2, 512], F32, "mb") for _ in range(2)]
        mo = [K.sb([2, 512], F32, "mo") for _ in range(2)]
        pi = 0
        for l in range(2):
            Wv = K.mod_w[l].rearrange("(p k) n -> p k n", k=32)
            for nb in range(48):
                ps = K.PS[nb % 2]
                for kp in range(4):
                    ws = wst[pi % 3]
                    S.dma("sp", ws.t[:, :, :], Wv[:, kp * 8:(kp + 1) * 8, nb * 512:(nb + 1) * 512], writes=[ws])
                    for c in range(8):
                        S.pe(lambda e: e.matmul(ps.t[0:2, :], lhsT=sc.t[:, :, kp * 8 + c], rhs=ws.t[:, c, :],
                                                start=(kp == 0 and c == 0), stop=(kp == 3 and c == 7)), [sc, ws], [ps])
                    pi += 1
                b = mb[nb % 2]
                o = mo[nb % 2]
                S.dma("act", b.t[:, :], K.mod_b[l:l + 1, nb * 512:(nb + 1) * 512].partition_broadcast(2), writes=[b])
                S.dve(lambda e: e.tensor_tensor(out=o.t[:, :], in0=ps.t[0:2, :], in1=b.t[:, :], op=ALU.add), [ps, b], [o])
                S.dma("act", K.MV[l, :, nb * 512:(nb + 1) * 512], o.t[:, :], reads=[o])


GROUPS = [[0, 1]] + [[2 + 4 * g + i for i in range(4)] for g in range(8)]
GROUPS8 = [[0, 1]] + [[2 + 8 * g + i for i in range(8)] for g in range(4)]


def lin_bufs(K, nws=2, nwb=2):
    wst = [K.sb([128, 8, 512], F32, "wst") for _ in range(nws)]
    wbf = [K.sb([128, 8, 512], BF16, "wbf") for _ in range(nwb)]
    return wst, wbf


def norm_bufs(K):
    return (K.sb([128, 1], F32, "ss"), K.sb([128, 1], F32, "rs"), K.sb([128, D], BF16, "junk"), K.sb([128, 128], F32, "dg"))


def stage_inproj(K, l, W, ncols, OUT, gain_ap, ctx_cols=None):
    S = K.S
    with K.stage():
        G1l, SHl = mod_tables(K, l, 0, 0, 1, gain_ap, "l")
        G1c, SHc = mod_tables(K, l, 1, 0, 1, gain_ap, "c")
        xT = K.sb([128, 32, 1024], BF16, "xT")
        xt = [K.sb([128, D], F32, "xt") for _ in range(2)]
        nb_ = norm_bufs(K)
        wb_ = lin_bufs(K, 4, 2)
        ev = [K.sb([128, 512], F32, "ev") for _ in range(3)]
        evi = [0]
        blocks_all = [(n0, min(512, ncols - n0)) for n0 in range(0, ncols, 512)]
        xi = 0
        for grp in GROUPS8:
            isctx = grp[0] < 2
            for ti, t in enumerate(grp):
                x = xt[xi % 2]
                xi += 1
                S.dma("sp", x.t[:, :], K.H[t * 128:(t + 1) * 128, :], writes=[x])
                norm_T(K, x, G1c if isctx else G1l, SHc if isctx else SHl, xT, ti * 128, nb_)
            blocks = blocks_all
            if isctx and ctx_cols is not None:
                blocks = [b for b in blocks_all if b[0] >= ctx_cols[0] and b[0] < ctx_cols[1]]

            def epi(ti, bi, n0, n, ps, grp=grp):
                e_ = ev[evi[0] % 3]
                evi[0] += 1
                S.act(lambda e: e.copy(out=e_.t[:, 0:n], in_=ps.t[:, 0:n]), [ps], [e_])
                t = grp[ti]
                S.dma("act", OUT[t * 128:(t + 1) * 128, n0:n0 + n], e_.t[:, 0:n], reads=[e_])

            linear_tok(K, xT, [(i * 128, 128) for i in range(len(grp))], W, D, blocks, epi, wb_, cast_engs=("dve", "act"))


def stage_gla(K):
    S = K.S
    nc = K.nc
    with K.stage():
        cst = {}
        for i, nm in enumerate(["A1f", "A3f", "A1b", "A3b", "MKf", "MKb"]):
            cst[nm] = K.sb([128, 128], F32, nm)
            S.dma("sp", cst[nm].t[:, :], K.cmat[i], writes=[cst[nm]])
        CH = K.sb([128, 2], F32, "CH")
        S.dma("sp", CH.t[:, :], K.cch[:, :], writes=[CH])
        GW = []
        for d in range(2):
            g = K.sb([33, 1024], F32, "GW")
            S.dve(lambda e: e.memset(g.t[:, :], 0.0), [], [g])
            S.dma("sp", g.t[16 * d:16 * d + 16, :], K.gla_gate_w[d], writes=[g])
            S.dma("sp", g.t[32:33, :], K.gla_gate_b[d:d + 1, :], writes=[g])
            GW.append(g)
        St32 = [[K.sb([128, 2, 512], F32, "S32") for h in range(4)] for d in range(2)]
        St16 = [[K.sb([128, 2, 512], BF16, "S16") for h in range(4)] for d in range(2)]
        for d in range(2):
            for h in range(4):
                S.dve(lambda e: e.memset(St32[d][h].t[:, :, :], 0.0), [], [St32[d][h]])
                S.pool(lambda e: e.memset(St16[d][h].t[:, :, :], 0.0), [], [St16[d][h]])
        B = []
        for d in range(2):
            b = dict(
                q32=K.sb([128, 1024], F32, "q32"), k32=K.sb([128, 1024], F32, "k32"), v32=K.sb([128, 2048], F32, "v32"),
                lr=K.sb([128, 32], F32, "lr"), v16=K.sb([128, 2048], BF16, "v16"), lrT=K.sb([33, 128], F32, "lrT"),
                l32=K.sb([128, 1024], F32, "l32"), e1=K.sb([128, 1024], F32, "e1"), e2=K.sb([128, 1024], F32, "e2"),
                e3=K.sb([128, 1024], F32, "e3"), kdec=K.sb([128, 1024], BF16, "kdec"), dec=K.sb([128, 16], F32, "dec"),
                qinT=K.sb([128, 8, 128], BF16, "qinT"), kinT=K.sb([128, 8, 128], BF16, "kinT"),
                sT=K.sb([128, 128], BF16, "sT"), o32=K.sb([128, 2048], F32, "o32"))
            S.dve(lambda e: e.memset(b["lrT"].t[:, :], 1.0), [], [b["lrT"]])
            B.append(b)
        PS = K.PS
        order = [[0, 1] + list(range(2, 34)), [1, 0] + list(range(33, 1, -1))]
        for step in range(NT):
            for d in range(2):
                t = order[d][step]
                b = B[d]
                r0 = t * 128
                A1 = cst["A1f"] if d == 0 else cst["A1b"]
                A3 = cst["A3f"] if d == 0 else cst["A3b"]
                MK = cst["MKf"] if d == 0 else cst["MKb"]
                q32, k32, v32, lr, v16, lrT, l32 = b["q32"], b["k32"], b["v32"], b["lr"], b["v16"], b["lrT"], b["l32"]
                e1, e2, e3, kdec, dec, qinT, kinT, sT, o32 = b["e1"], b["e2"], b["e3"], b["kdec"], b["dec"], b["qinT"], b["kinT"], b["sT"], b["o32"]
                S.dma("sp", q32.t[:, :], K.P0[r0:r0 + 128, 0:1024], writes=[q32])
                S.dma("sp", k32.t[:, :], K.P0[r0:r0 + 128, 1024:2048], writes=[k32])
                S.dma("sp", v32.t[:, :], K.P0[r0:r0 + 128, 2048:4096], writes=[v32])
                S.dma("sp", lr.t[:, :], K.P0[r0:r0 + 128, 6144:6176], writes=[lr])
                S.pool(lambda e: e.tensor_copy(out=v16.t[:, :], in_=v32.t[:, :]), [v32], [v16])
                S.pe(lambda e: e.matmul(PS[4].t[0:32, 128:256], lhsT=lr.t[:, 0:32], rhs=K.ident.t[:, :], start=True, stop=True),
                     [lr, K.ident], [PS[4]])
                S.dve(lambda e: e.tensor_copy(out=lrT.t[0:32, :], in_=PS[4].t[0:32, 128:256]), [PS[4]], [lrT])
                for blk in range(2):
                    S.pe(lambda e: e.matmul(PS[blk].t[:, :], lhsT=lrT.t[0:33, :], rhs=GW[d].t[0:33, blk * 512:(blk + 1) * 512],
                                            start=True, stop=True), [lrT, GW[d]], [PS[blk]])
                    S.act(lambda e: e.activation(out=l32.t[:, blk * 512:(blk + 1) * 512], in_=PS[blk].t[:, :], func=AF.Exp, scale=-1.0),
                          [PS[blk]], [l32])
                S.act(lambda e: e.activation(out=l32.t[:, :], in_=l32.t[:, :], func=AF.Ln, bias=1.0), [l32], [l32])
                for blk in range(2):
                    S.pe(lambda e: e.matmul(PS[blk].t[:, :], lhsT=A1.t[:, :], rhs=l32.t[:, blk * 512:(blk + 1) * 512], start=True, stop=True),
                         [A1, l32], [PS[blk]])
                    S.pe(lambda e: e.matmul(PS[2 + blk].t[:, :], lhsT=A3.t[:, :], rhs=l32.t[:, blk * 512:(blk + 1) * 512], start=True, stop=True),
                         [A3, l32], [PS[2 + blk]])
                    sl = slice(blk * 512, (blk + 1) * 512)
                    S.act(lambda e: e.activation(out=e1.t[:, sl], in_=PS[blk].t[:, :], func=AF.Exp), [PS[blk]], [e1])
                    S.act(lambda e: e.activation(out=e2.t[:, sl], in_=PS[blk].t[:, :], func=AF.Exp, scale=-1.0), [PS[blk]], [e2])
                    S.act(lambda e: e.activation(out=e3.t[:, sl], in_=PS[2 + blk].t[:, :], func=AF.Exp), [PS[2 + blk]], [e3])
                S.dve(lambda e: e.scalar_tensor_tensor(out=e1.t[:, :], in0=q32.t[:, :], scalar=1.0 / 16.0, in1=e1.t[:, :],
                                                       op0=ALU.mult, op1=ALU.mult), [q32, e1], [e1])
                S.dve(lambda e: e.tensor_tensor(out=e2.t[:, :], in0=k32.t[:, :], in1=e2.t[:, :], op=ALU.mult), [k32, e2], [e2])
                S.pool(lambda e: e.tensor_tensor(out=kdec.t[:, :], in0=k32.t[:, :], in1=e3.t[:, :], op=ALU.mult), [k32, e3], [kdec])
                for db in range(8):
                    S.pe(lambda e: e.matmul(PS[4].t[:, db * 2:db * 2 + 2], lhsT=l32.t[:, db * 128:(db + 1) * 128], rhs=CH.t[:, 0:2],
                                            start=True, stop=True), [l32, CH], [PS[4]])
                S.act(lambda e: e.activation(out=dec.t[:, 0:16], in_=PS[4].t[:, 0:16], func=AF.Exp), [PS[4]], [dec])
                transpose_blocks(K, e1, 8, qinT, 0, eng="act")
                transpose_blocks(K, e2, 8, kinT, 0, eng="dve")
                for h in range(4):
                    s32, s16 = St32[d][h], St16[d][h]
                    for m in range(2):
                        S.pe(lambda e: e.matmul(PS[5].t[:, 0:128], lhsT=kinT.t[:, 2 * h + m, :], rhs=qinT.t[:, 2 * h + m, :],
                                                start=(m == 0), stop=(m == 1)), [kinT, qinT], [PS[5]])
                    S.dve(lambda e: e.tensor_tensor(out=sT.t[:, :], in0=PS[5].t[:, 0:128], in1=MK.t[:, :], op=ALU.mult), [PS[5], MK], [sT])
                    pso = PS[6 + h % 2]
                    S.pe(lambda e: e.matmul(pso.t[:, :], lhsT=sT.t[:, :], rhs=v16.t[:, h * 512:(h + 1) * 512], start=True, stop=False),
                         [sT, v16], [pso])
                    chunks = [0, 1] if d == 0 else [1, 0]
                    for idx, ci in enumerate(chunks):
                        rows = slice(ci * 64, ci * 64 + 64)
                        for m in range(2):
                            S.pe(lambda e: e.matmul(pso.t[rows, :], lhsT=qinT.t[:, 2 * h + m, rows], rhs=s16.t[:, m, :],
                                                    start=False, stop=(idx == 1 and m == 1)), [qinT, s16], [pso])
                        for m in range(2):
                            psu = PS[2 + m]
                            S.pe(lambda e: e.matmul(psu.t[:, :], lhsT=kdec.t[rows, (2 * h + m) * 128:(2 * h + m + 1) * 128],
                                                    rhs=v16.t[rows, h * 512:(h + 1) * 512], start=True, stop=True), [kdec, v16], [psu])
                            dcol = (2 * h + m) * 2 + ci
                            S.dve(lambda e: e.scalar_tensor_tensor(out=s32.t[:, m, :], in0=s32.t[:, m, :], scalar=dec.t[:, dcol:dcol + 1],
                                                                   in1=psu.t[:, :], op0=ALU.mult, op1=ALU.add), [s32, dec, psu], [s32])
                            S.act(lambda e: e.copy(out=s16.t[:, m, :], in_=s32.t[:, m, :]), [s32], [s16])
                    S.act(lambda e: e.copy(out=o32.t[:, h * 512:(h + 1) * 512], in_=pso.t[:, :]), [pso], [o32])
                S.dma("act", K.OG[d, r0:r0 + 128, :], o32.t[:, :], reads=[o32])


def load_bc(K, vec_ap, n, name):
    t = K.sb([128, n], F32, name)
    K.S.dma("sp", t.t[:, :], vec_ap.partition_broadcast(128), writes=[t])
    return t


def stage_merge(K, l):
    S = K.S
    PS = K.PS
    with K.stage():
        gnbc = load_bc(K, K.gla_norm[0:1, :], 2048, "gnbc")
        nwbc = load_bc(K, K.sg_norm_w[0:1, :], 2048, "nwbc")
        nbbc = load_bc(K, K.sg_norm_b[0:1, :], 2048, "nbbc")
        wsT = K.sb([128, 4, 128], BF16, "wsT")
        wtmp = K.sb([128, 512], F32, "wtmp")
        S.dma("sp", wtmp.t[:, :].rearrange("p (g j) -> p g j", g=4), K.sg_ws.rearrange("g i j -> i g j"), writes=[wtmp])
        transpose_blocks(K, wtmp, 4, wsT, 0, eng="dve")
        bsT = K.sb([128, 4], F32, "bsT")
        btmp = K.sb([4, 128], F32, "btmp")
        S.dma("sp", btmp.t[:, :], K.sg_bs[:, :], writes=[btmp])
        S.pe(lambda e: e.matmul(PS[7].t[:, 0:4], lhsT=btmp.t[0:4, :], rhs=K.ident.t[0:4, 0:4], start=True, stop=True), [btmp, K.ident], [PS[7]])
        S.dve(lambda e: e.tensor_copy(out=bsT.t[:, :], in_=PS[7].t[:, 0:4]), [PS[7]], [bsT])
        sets = []
        for _ in range(2):
            sets.append(dict(of=K.sb([128, 2048], F32, "of"), ob=K.sb([128, 2048], F32, "ob"), g32=K.sb([128, 2048], F32, "g32"),
                             u32=K.sb([128, 2048], F32, "u32"), vs32=K.sb([128, 2048], F32, "vs32"),
                             mTn=K.sb([128, 32, 128], BF16, "mTn"), st4=[K.sb([128, 4], F32, "st4") for _ in range(4)]))
        mrg = K.sb([128, D], F32, "mrg")
        vn16 = K.sb([128, 2048], BF16, "vn16")
        junk = K.sb([128, 512], BF16, "junk")
        XTv = K.XT.rearrange("(c p) t -> p c t", p=128)
        for grp in [list(range(NT))]:
            for ti, t in enumerate(grp):
                r0 = t * 128
                st_ = sets[t % 2]
                of, ob, g32, u32, vs32, mTn, st4 = st_["of"], st_["ob"], st_["g32"], st_["u32"], st_["vs32"], st_["mTn"], st_["st4"]
                S.dma("sp", of.t[:, :], K.OG[0, r0:r0 + 128, :], writes=[of])
                S.dma("sp", ob.t[:, :], K.OG[1, r0:r0 + 128, :], writes=[ob])
                S.dma("sp", g32.t[:, :], K.P0[r0:r0 + 128, 4096:6144], writes=[g32])
                S.dma("sp", u32.t[:, :], K.P0[r0:r0 + 128, 6176:8224], writes=[u32])
                S.dma("sp", vs32.t[:, :], K.P0[r0:r0 + 128, 8224:10272], writes=[vs32])
                ss, rs, mu, var = st4
                S.dve(lambda e: e.tensor_tensor(out=of.t[:, :], in0=of.t[:, :], in1=ob.t[:, :], op=ALU.add), [of, ob], [of])
                S.dve(lambda e: e.memset(ss.t[:, :], 0.0), [], [ss])
                for h in range(4):
                    S.act(lambda e: e.activation(out=junk.t[:, :], in_=of.t[:, h * 512:(h + 1) * 512], func=AF.Square,
                                                 accum_out=ss.t[:, h:h + 1]), [of, ss], [junk, ss])
                S.act(lambda e: e.activation(out=rs.t[:, :], in_=ss.t[:, :], func=AF.Sqrt, scale=1.0 / 512, bias=EPS), [ss], [rs])
                S.dve(lambda e: e.reciprocal(out=rs.t[:, :], in_=rs.t[:, :]), [rs], [rs])
                for h in range(4):
                    sl = slice(h * 512, (h + 1) * 512)
                    S.dve(lambda e: e.scalar_tensor_tensor(out=of.t[:, sl], in0=of.t[:, sl], scalar=rs.t[:, h:h + 1], in1=gnbc.t[:, sl],
                                                           op0=ALU.mult, op1=ALU.mult), [of, rs, gnbc], [of])
                S.act(lambda e: e.activation(out=g32.t[:, :], in_=g32.t[:, :], func=AF.Silu), [g32], [g32])
                S.dve(lambda e: e.tensor_tensor(out=mrg.t[:, 0:2048], in0=of.t[:, :], in1=g32.t[:, :], op=ALU.mult), [of, g32], [mrg])
                S.act(lambda e: e.activation(out=u32.t[:, :], in_=u32.t[:, :], func=AF.Gelu_apprx_tanh), [u32], [u32])
                S.act(lambda e: e.activation(out=vs32.t[:, :], in_=vs32.t[:, :], func=AF.Gelu_apprx_tanh), [vs32], [vs32])
                S.dve(lambda e: e.tensor_reduce(out=mu.t[:, :], in_=vs32.t[:, :].rearrange("p (g c) -> p g c", g=4), axis=AX.X, op=ALU.add),
                      [vs32], [mu])
                S.dve(lambda e: e.tensor_scalar_mul(out=mu.t[:, :], in0=mu.t[:, :], scalar1=-1.0 / 512), [mu], [mu])
                S.dve(lambda e: e.memset(var.t[:, :], 0.0), [], [var])
                for g in range(4):
                    sl = slice(g * 512, (g + 1) * 512)
                    S.act(lambda e: e.activation(out=vs32.t[:, sl], in_=vs32.t[:, sl], func=AF.Identity, bias=mu.t[:, g:g + 1], scale=1.0),
                          [vs32, mu], [vs32])
                    S.act(lambda e: e.activation(out=junk.t[:, :], in_=vs32.t[:, sl], func=AF.Square, accum_out=var.t[:, g:g + 1]),
                          [vs32, var], [junk, var])
                S.act(lambda e: e.activation(out=var.t[:, :], in_=var.t[:, :], func=AF.Sqrt, scale=1.0 / 512, bias=EPS), [var], [var])
                S.dve(lambda e: e.reciprocal(out=var.t[:, :], in_=var.t[:, :]), [var], [var])
                for g in range(4):
                    sl = slice(g * 512, (g + 1) * 512)
                    S.dve(lambda e: e.scalar_tensor_tensor(out=vs32.t[:, sl], in0=vs32.t[:, sl], scalar=var.t[:, g:g + 1], in1=nwbc.t[:, sl],
                                                           op0=ALU.mult, op1=ALU.mult), [vs32, var, nwbc], [vs32])
                    S.pool(lambda e: e.tensor_tensor(out=vn16.t[:, sl], in0=vs32.t[:, sl], in1=nbbc.t[:, sl], op=ALU.add), [vs32, nbbc], [vn16])
                    ps = PS[4 + g % 2]
                    S.pe(lambda e: e.matmul(ps.t[:, :], lhsT=wsT.t[:, g, :], rhs=vn16.t[:, sl], start=True, stop=True), [wsT, vn16], [ps])
                    S.dve(lambda e: e.scalar_tensor_tensor(out=mrg.t[:, 2048 + g * 512:2048 + (g + 1) * 512], in0=ps.t[:, :],
                                                           scalar=bsT.t[:, g:g + 1], in1=u32.t[:, sl], op0=ALU.add, op1=ALU.mult),
                          [ps, bsT, u32], [mrg])
                transpose_blocks(K, mrg, 32, mTn, 0, eng="act")
                S.dma("act", XTv[:, :, r0:r0 + 128], mTn.t[:, :, :], reads=[mTn])
    stage_outproj(K, l, K.ab_w_out, list(range(NT)))


def stage_outproj(K, l, W, tiles_all):
    S = K.S
    with K.stage():
        m2 = [load_bc(K, K.MV[l, r:r + 1, 2 * D:3 * D], D, "m2") for r in range(2)]
        xT = K.sb([128, 32, 1024], BF16, "xT")
        wb_ = lin_bufs(K, 4, 2)
        hb = [K.sb([128, 512], F32, "hb") for _ in range(6)]
        hi = [0]
        XTv = K.XT.rearrange("(c p) t -> p c t", p=128)
        groups = []
        if tiles_all[0] < 2:
            groups.append([0, 1])
        lat = [t for t in tiles_all if t >= 2]
        for i in range(0, len(lat), 8):
            groups.append(lat[i:i + 8])
        for grp in groups:
            isctx = grp[0] < 2
            n = len(grp) * 128
            S.dma("sp", xT.t[:, :, 0:n], XTv[:, :, grp[0] * 128:grp[0] * 128 + n], writes=[xT])
            mm = m2[1] if isctx else m2[0]

            def epi(ti, bi, n0, nn, ps, grp=grp, mm=mm):
                h_ = hb[hi[0] % 6]
                hi[0] += 1
                t = grp[ti]
                S.dma("pool", h_.t[:, 0:nn], K.H[t * 128:(t + 1) * 128, n0:n0 + nn], writes=[h_])
                S.dve(lambda e: e.tensor_tensor(out=ps.t[:, 0:nn], in0=ps.t[:, 0:nn], in1=mm.t[:, n0:n0 + nn], op=ALU.mult), [ps, mm], [ps])
                S.dve(lambda e: e.tensor_tensor(out=h_.t[:, 0:nn], in0=h_.t[:, 0:nn], in1=ps.t[:, 0:nn], op=ALU.add), [h_, ps], [h_])
                S.dma("act", K.H[t * 128:(t + 1) * 128, n0:n0 + nn], h_.t[:, 0:nn], reads=[h_])

            linear_tok(K, xT, [(i * 128, 128) for i in range(len(grp))], W, D, [(n0, 512) for n0 in range(0, D, 512)], epi, wb_, cast_engs=("act", "dve"))


def stage_moe(K, l, with_ctx):
    S = K.S
    PS = K.PS
    rows = [1, 0] if with_ctx else [0]
    with K.stage():
        IDXT = K.sb([128, 4, 16], I32, "IDXT")
        GATE = K.sb([128, 4, 16], F32, "GATE")
        IDXTC = K.sb([32, 16], I32, "IDXTC")
        GATEC = K.sb([32, 16], F32, "GATEC")
        IDX8 = K.sb([128, 4, 16], I32, "IDX8")
        IDX8C = K.sb([32, 16], I32, "IDX8C")
        afs = ExitStack()
        old_st = K.st
        K.st = afs
        AFFT = K.sb([16, NTOK], F32, "AFFT")
        K.st = old_st
        with K.stage():
            gbc = load_bc(K, K.norm_ffn[l:l + 1, :], D, "gbc")
            G1 = K.sb([128, D], F32, "G1bc")
            SH = K.sb([128, D], F32, "SHbc")
            RW = K.sb([128, 32, 16], F32, "RW")
            S.dma("sp", RW.t[:, :, :], K.router_w[l].rearrange("(c p) e -> p c e", p=128), writes=[RW])
            xt = [K.sb([128, D], F32, "xt") for _ in range(2)]
            hm = K.sb([128, D], F32, "hm")
            hmT = K.sb([128, 32, 128], F32, "hmT")
            hm16 = K.sb([128, D], BF16, "hm16")
            ss, rs, junk, dg = norm_bufs(K)
            sm = [K.sb([128, 16], F32, "sm") for _ in range(6)]
            xi = 0
            for r in rows:
                S.dma("sp", G1.t[:, :], K.MV[l, r:r + 1, 4 * D:5 * D].partition_broadcast(128), writes=[G1])
                S.dma("sp", SH.t[:, :], K.MV[l, r:r + 1, 3 * D:4 * D].partition_broadcast(128), writes=[SH])
                S.dve(lambda e: e.scalar_tensor_tensor(out=G1.t[:, :], in0=G1.t[:, :], scalar=1.0, in1=gbc.t[:, :], op0=ALU.add, op1=ALU.mult),
                      [G1, gbc], [G1])
                for t in ([0, 1] if r == 1 else range(2, NT)):
                    x = xt[xi % 2]
                    xi += 1
                    S.dma("sp", x.t[:, :], K.H[t * 128:(t + 1) * 128, :], writes=[x])
                    rstd_of(K, x, D, ss, rs, junk, 1.0 / D)
                    S.dve(lambda e: e.scalar_tensor_tensor(out=hm.t[:, :], in0=x.t[:, :], scalar=rs.t[:, 0:1], in1=G1.t[:, :],
                                                           op0=ALU.mult, op1=ALU.mult), [x, rs, G1], [hm])
                    S.pool(lambda e: e.tensor_tensor(out=hm.t[:, :], in0=hm.t[:, :], in1=SH.t[:, :], op=ALU.add), [hm, SH], [hm])
                    S.act(lambda e: e.copy(out=hm16.t[:, :], in_=hm.t[:, :]), [hm], [hm16])
                    S.dma("act", K.HM[t * 128:(t + 1) * 128, :], hm16.t[:, :], reads=[hm16])
                    transpose_blocks(K, hm, 32, hmT, 0, eng="act")
                    for c in range(32):
                        S.pe(lambda e: e.matmul(PS[5].t[:, 0:16], lhsT=hmT.t[:, c, :], rhs=RW.t[:, c, :], start=(c == 0), stop=(c == 31)),
                             [hmT, RW], [PS[5]])
                    mx, nmx, sme, ex, rsm, aff = sm
                    S.dve(lambda e: e.tensor_reduce(out=mx.t[:, 0:1], in_=PS[5].t[:, 0:16], axis=AX.X, op=ALU.max), [PS[5]], [mx])
                    S.dve(lambda e: e.tensor_scalar_mul(out=nmx.t[:, 0:1], in0=mx.t[:, 0:1], scalar1=-1.0), [mx], [nmx])
                    S.dve(lambda e: e.memset(sme.t[:, 0:1], 0.0), [], [sme])
                    S.act(lambda e: e.activation(out=ex.t[:, :], in_=PS[5].t[:, 0:16], func=AF.Exp, bias=nmx.t[:, 0:1], scale=1.0,
                                                 accum_out=sme.t[:, 0:1]), [PS[5], nmx, sme], [ex, sme])
                    S.dve(lambda e: e.reciprocal(out=rsm.t[:, 0:1], in_=sme.t[:, 0:1]), [sme], [rsm])
                    S.dve(lambda e: e.tensor_scalar_mul(out=aff.t[:, :], in0=ex.t[:, :], scalar1=rsm.t[:, 0:1]), [ex, rsm], [aff])
                    S.pe(lambda e: e.matmul(PS[4].t[0:16, 0:128], lhsT=aff.t[:, 0:16], rhs=K.ident.t[:, :], start=True, stop=True),
                         [aff, K.ident], [PS[4]])
                    S.dve(lambda e: e.tensor_copy(out=AFFT.t[0:16, t * 128:(t + 1) * 128], in_=PS[4].t[0:16, 0:128]), [PS[4]], [AFFT])
        with K.stage():
            Wk = K.sb([16, T], F32, "Wk")
            VALS = K.sb([16, CAP + CAPC], F32, "VALS")
            IDX = K.sb([16, CAP + CAPC], U32, "IDX")
            S.dve(lambda e: e.tensor_copy(out=Wk.t[:, :], in_=AFFT.t[:, L:NTOK]), [AFFT], [Wk])
            for r in range(CAP // 8):
                sl = slice(r * 8, r * 8 + 8)
                S.dve(lambda e: e.max(out=VALS.t[:, sl], in_=Wk.t[:, :]), [Wk], [VALS])
                S.dve(lambda e: e.max_index(out=IDX.t[:, sl], in_max=VALS.t[:, sl], in_values=Wk.t[:, :]), [Wk, VALS], [IDX])
                S.dve(lambda e: e.match_replace(out=Wk.t[:, :], in_to_replace=VALS.t[:, sl], in_values=Wk.t[:, :], imm_value=-1.0),
                      [Wk, VALS], [Wk])
            if with_ctx:
                S.dve(lambda e: e.tensor_copy(out=Wk.t[:, 0:L], in_=AFFT.t[:, 0:L]), [AFFT, Wk], [Wk])
                for r in range(CAPC // 8):
                    sl = slice(CAP + r * 8, CAP + r * 8 + 8)
                    S.dve(lambda e: e.max(out=VALS.t[:, sl], in_=Wk.t[:, 0:L]), [Wk], [VALS])
                    S.dve(lambda e: e.max_index(out=IDX.t[:, sl], in_max=VALS.t[:, sl], in_values=Wk.t[:, 0:L]), [Wk, VALS], [IDX])
                    S.dve(lambda e: e.match_replace(out=Wk.t[:, 0:L], in_to_replace=VALS.t[:, sl], in_values=Wk.t[:, 0:L], imm_value=-1.0),
                          [Wk, VALS], [Wk])
            else:
                S.dve(lambda e: e.memset(VALS.t[:, CAP:CAP + CAPC], 0.0), [], [VALS])
                S.dve(lambda e: e.memset(IDX.t[:, CAP:CAP + CAPC], 0), [], [IDX])
            bI = Buf("idxd")
            bV = Buf("vald")
            S.dma("sp", K.IDXD[l, :, :], IDX.t[:, :].bitcast(I32), reads=[IDX], writes=[bI])
            S.dma("sp", K.VALD[l, :, :], VALS.t[:, :], reads=[VALS], writes=[bV])
            for j in range(4):
                S.dma("sp", IDXT.t[:, j, :], K.IDXD[l, :, j * 128:(j + 1) * 128].rearrange("e p -> p e"), reads=[bI], writes=[IDXT],
                      allow_slow_non_contiguous=True)
                S.dma("sp", GATE.t[:, j, :], K.VALD[l, :, j * 128:(j + 1) * 128].rearrange("e p -> p e"), reads=[bV], writes=[GATE],
                      allow_slow_non_contiguous=True)
            S.dma("sp", IDXTC.t[:, :], K.IDXD[l, :, CAP:CAP + CAPC].rearrange("e p -> p e"), reads=[bI], writes=[IDXTC],
                  allow_slow_non_contiguous=True)
            S.dma("sp", GATEC.t[:, :], K.VALD[l, :, CAP:CAP + CAPC].rearrange("e p -> p e"), reads=[bV], writes=[GATEC],
                  allow_slow_non_contiguous=True)
            S.dve(lambda e: e.tensor_single_scalar(out=IDX8.t[:, :, :], in_=IDXT.t[:, :, :], scalar=3, op=ALU.logical_shift_left), [IDXT], [IDX8])
            S.dve(lambda e: e.tensor_single_scalar(out=IDX8C.t[:, :], in_=IDXTC.t[:, :], scalar=3, op=ALU.logical_shift_left), [IDXTC], [IDX8C])
            S.dma("sp", K.DBG8[l, :, :], IDX8.t[:, :, :].rearrange("p j e -> p (j e)"), reads=[IDX8])
        S.barrier()
        afs.close()
        with K.stage():
            m5 = [load_bc(K, K.MV[l, r:r + 1, 5 * D:6 * D], D, "m5") for r in ([0, 1] if with_ctx else [0])]
            ntile = 5 if with_ctx else 4
            xg = [K.sb([128, D], BF16, "xg") for _ in range(ntile)]
            ncol = CAP + (CAPC if with_ctx else 0)
            xgT = K.sb([128, 32, ncol], BF16, "xgT")
            gs = K.sb([128, 4, ncol], F32, "gs")
            hidT = K.sb([128, 8, ncol], BF16, "hidT")
            wst, wbf = lin_bufs(K, 3, 2)
            ysb = [K.sb([128, 512], F32, "ysb") for _ in range(3)]
            id16 = K.sb([128, 128], BF16, "id16")
            S.dve(lambda e: e.tensor_copy(out=id16.t[:, :], in_=K.ident.t[:, :]), [K.ident], [id16])
            yi = [0]
            HCB = [Buf("hcb%d" % i) for i in range(8)]
            H8 = K.H.rearrange("t (nb c) -> (t nb) c", c=512)
            ce = ("dve", "act")
            pi = 0

            def gather_expert(ex_):
                for j in range(4):
                    S.idma(xg[j].t[:, :], None, K.HM[:, :], bass.IndirectOffsetOnAxis(ap=IDXT.t[:, j, ex_:ex_ + 1], axis=0),
                           reads=[IDXT], writes=[xg[j]], element_offset=L * D)
                if with_ctx:
                    S.idma(xg[4].t[0:CAPC, :], None, K.HM[:, :], bass.IndirectOffsetOnAxis(ap=IDXTC.t[0:CAPC, ex_:ex_ + 1], axis=0),
                           reads=[IDXTC], writes=[xg[4]])

            def transposes_expert(ex_):
                for j in range(4):
                    transpose_blocks(K, xg[j], 32, xgT, j * 128, eng="act" if j % 2 == 0 else "dve", ident=id16)
                if with_ctx:
                    transpose_blocks(K, xg[4], 32, xgT, CAP, m=CAPC, eng="act", ident=id16)

            gather_expert(0)
            transposes_expert(0)
            for ex_ in range(NEXP):
                if ex_ + 1 < NEXP:
                    gather_expert(ex_ + 1)
                for half in range(2):
                    for wi, Wm in enumerate((K.moe_w_gate[l, ex_], K.moe_w_up[l, ex_])):
                        for kp in range(4):
                            ws, wb = wst[pi % 3], wbf[pi % 2]
                            src = Wm[kp * 1024:(kp + 1) * 1024, half * 512:(half + 1) * 512].rearrange("(c p) n -> p c n", p=128)
                            S.dma("sp", ws.t[:, :, :], src, writes=[ws])
                            _cast(S, ce[pi % 2], wb.t[:, :, :], ws.t[:, :, :], ws, wb)
                            pi += 1
                            for c in range(8):
                                first = (kp == 0 and c == 0)
                                lastk = (kp == 3 and c == 7)
                                for fb in range(4):
                                    S.pe(lambda e: e.matmul(PS[fb].t[:, 0:CAP], lhsT=wb.t[:, c, fb * 128:(fb + 1) * 128],
                                                            rhs=xgT.t[:, kp * 8 + c, 0:CAP], start=first, stop=lastk), [wb, xgT], [PS[fb]])
                                    if with_ctx:
                                        S.pe(lambda e: e.matmul(PS[4 + fb].t[:, 0:CAPC], lhsT=wb.t[:, c, fb * 128:(fb + 1) * 128],
                                                                rhs=xgT.t[:, kp * 8 + c, CAP:ncol], start=first, stop=lastk), [wb, xgT], [PS[4 + fb]])
                        for fb in range(4):
                            if wi == 0:
                                S.act(lambda e: e.activation(out=gs.t[:, fb, 0:CAP], in_=PS[fb].t[:, 0:CAP], func=AF.Silu), [PS[fb]], [gs])
                            else:
                                S.dve(lambda e: e.tensor_tensor(out=hidT.t[:, half * 4 + fb, 0:CAP], in0=gs.t[:, fb, 0:CAP], in1=PS[fb].t[:, 0:CAP],
                                                                op=ALU.mult), [gs, PS[fb]], [hidT])
                        if with_ctx:
                            for fb in range(4):
                                pv = PS[4 + fb].t[:, 0:CAPC]
                                if wi == 0:
                                    S.act(lambda e: e.activation(out=gs.t[:, fb, CAP:ncol], in_=pv, func=AF.Silu), [PS[4 + fb]], [gs])
                                else:
                                    S.dve(lambda e: e.tensor_tensor(out=hidT.t[:, half * 4 + fb, CAP:ncol], in0=gs.t[:, fb, CAP:ncol], in1=pv,
                                                                    op=ALU.mult), [gs, PS[4 + fb]], [hidT])
                if ex_ + 1 < NEXP:
                    transposes_expert(ex_ + 1)

                def epi(ti, bi, n0, n, ps, ex_=ex_):
                    y = ysb[yi[0] % 3]
                    yi[0] += 1
                    if ti < 4:
                        S.dve(lambda e: e.scalar_tensor_tensor(out=y.t[:, :], in0=ps.t[:, :], scalar=GATE.t[:, ti, ex_:ex_ + 1], in1=m5[0].t[:, n0:n0 + n],
                                                               op0=ALU.mult, op1=ALU.mult), [ps, GATE, m5[0]], [y])
                        S.idma(H8[:, :], bass.IndirectOffsetOnAxis(ap=IDX8.t[:, ti, ex_:ex_ + 1], axis=0), y.t[:, :], None,
                               reads=[y, IDX8], writes=[HCB[bi]], nowaw=(ti > 0), compute_op=ALU.add, element_offset=L * D + n0)
                    else:
                        S.dve(lambda e: e.scalar_tensor_tensor(out=y.t[0:CAPC, :], in0=ps.t[0:CAPC, :], scalar=GATEC.t[0:CAPC, ex_:ex_ + 1],
                                                               in1=m5[1].t[0:CAPC, n0:n0 + n], op0=ALU.mult, op1=ALU.mult), [ps, GATEC, m5[1]], [y])
                        S.idma(H8[:, :], bass.IndirectOffsetOnAxis(ap=IDX8C.t[0:CAPC, ex_:ex_ + 1], axis=0), y.t[0:CAPC, :], None,
                               reads=[y, IDX8C], writes=[HCB[bi]], nowaw=True, compute_op=ALU.add, element_offset=n0)

                tl = [(i * 128, 128) for i in range(4)] + ([(CAP, CAPC)] if with_ctx else [])
                K.lin_pi = pi
                linear_tok(K, hidT, tl, K.moe_w_down[l, ex_], FF, [(n0, 512) for n0 in range(0, D, 512)], epi, (wst, wbf), cast_engs=ce)
                pi = K.lin_pi


def rope(K, xs_ap, src, out, tmp, cos, sin):
    S = K.S
    S.dve(lambda e: e.tensor_tensor(out=tmp.t[:, :], in0=xs_ap, in1=cos.t[:, :], op=ALU.mult), [src, cos], [tmp])
    xv = xs_ap.rearrange("p (a b c) -> p a b c", b=2, c=32)
    sv = sin.t[:, :].rearrange("p (a b c) -> p a b c", b=2, c=32)
    ov = out.t[:, :].rearrange("p (a b c) -> p a b c", b=2, c=32)
    S.pool(lambda e: e.tensor_tensor(out=ov[:, :, 0, :], in0=xv[:, :, 1, :], in1=sv[:, :, 0, :], op=ALU.mult), [src, sin], [out])
    S.pool(lambda e: e.tensor_tensor(out=ov[:, :, 1, :], in0=xv[:, :, 0, :], in1=sv[:, :, 1, :], op=ALU.mult), [src, sin], [out])
    S.dve(lambda e: e.tensor_tensor(out=out.t[:, :], in0=out.t[:, :], in1=tmp.t[:, :], op=ALU.add), [out, tmp], [out])


def stage_attn(K, l=1):
    S = K.S
    PS = K.PS
    P1 = K.P1
    with K.stage():
        mk32 = K.sb([128, 512], F32, "mk32")
        MK = [K.sb([128, 512], BF16, "MK") for _ in range(2)]
        for i in range(2):
            S.dma("sp", mk32.t[:, :], K.amask[i], writes=[mk32])
            S.dve(lambda e: e.tensor_copy(out=MK[i].t[:, :], in_=mk32.t[:, :]), [mk32], [MK[i]])
        sk = K.sb([1, 32], F32, "sk")
        S.dma("sp", sk.t[:, :], K.sinks[0:1, :], writes=[sk])
        S.act(lambda e: e.activation(out=sk.t[:, :], in_=sk.t[:, :], func=AF.Exp), [sk], [sk])
        ones1 = K.sb([1, 128], F32, "ones1")
        S.dve(lambda e: e.memset(ones1.t[:, :], 1.0), [], [ones1])
        ESROW = K.sb([1, 32, 128], BF16, "ESROW")
        for h in range(32):
            S.dve(lambda e: e.tensor_scalar_mul(out=ESROW.t[0:1, h, :], in0=ones1.t[0:1, :], scalar1=sk.t[0:1, h:h + 1]), [ones1, sk], [ESROW])
        ONE1 = K.sb([1, 129], BF16, "ONE1")
        S.dve(lambda e: e.memset(ONE1.t[:, :], 0.0), [], [ONE1])
        S.dve(lambda e: e.memset(ONE1.t[:, 128:129], 1.0), [], [ONE1])
        KTC = K.sb([128, 8, 256], BF16, "KTC")
        VC = K.sb([128, 2, 8, 129], BF16, "VC")
        KT = [K.sb([128, 8, 128], BF16, "KT") for _ in range(4)]
        VA = [K.sb([128, 8, 129], BF16, "VA") for _ in range(4)]
        S.pool(lambda e: e.memset(VC.t[:, :, :, :], 1.0), [], [VC])
        for i in range(4):
            S.pool(lambda e: e.memset(VA[i].t[:, :, :], 1.0), [], [VA[i]])
        k32 = K.sb([128, 1024], F32, "k32")
        v32 = K.sb([128, 1024], F32, "v32")
        kr = K.sb([128, 1024], F32, "kr")
        tmp = K.sb([128, 1024], F32, "tmp")
        cosk = K.sb([128, 1024], F32, "cosk")
        sink = K.sb([128, 1024], F32, "sink")
        cosq = cosk
        sinq = sink
        q32 = K.sb([128, D], F32, "q32")
        QT = K.sb([128, 32, 128], BF16, "QT")
        p16 = [K.sb([128, 512], BF16, "p16") for _ in range(3)]
        aTn = [K.sb([128, 32, 128], BF16, "aTn") for _ in range(2)]
        XTv = K.XT.rearrange("(c p) t -> p c t", p=128)
        for tc in range(2):
            S.dma("sp", k32.t[:, :], P1[tc * 128:(tc + 1) * 128, 4096:5120], writes=[k32])
            transpose_blocks(K, k32, 8, KTC, tc * 128, eng="act")
            S.dma("sp", v32.t[:, :], P1[tc * 128:(tc + 1) * 128, 5120:6144], writes=[v32])
            S.pool(lambda e: e.tensor_copy(out=VC.t[:, tc, :, 0:128], in_=v32.t[:, :].rearrange("p (h d) -> p h d", d=128)), [v32], [VC])

        def prep_kv(t):
            slot = t % 4
            p0 = (t - 2) * 128
            S.dma("sp", k32.t[:, :], P1[t * 128:(t + 1) * 128, 4096:5120], writes=[k32])
            S.dma("sp", v32.t[:, :], P1[t * 128:(t + 1) * 128, 5120:6144], writes=[v32])
            S.dma("sp", cosk.t[:, :], K.cosk[p0:p0 + 128, :], writes=[cosk])
            S.dma("sp", sink.t[:, :], K.sink[p0:p0 + 128, :], writes=[sink])
            rope(K, k32.t[:, :], k32, kr, tmp, cosk, sink)
            transpose_blocks(K, kr, 8, KT[slot], 0, eng="act")
            S.pool(lambda e: e.tensor_copy(out=VA[slot].t[:, :, 0:128], in_=v32.t[:, :].rearrange("p (h d) -> p h d", d=128)), [v32], [VA[slot]])

        pcount = 0
        kc = 0
        ones16 = K.sb([128, 128], BF16, "ones16")
        S.dve(lambda e: e.memset(ones16.t[:, :], 1.0), [], [ones16])
        ones1b = K.sb([1, 128], BF16, "ones1b")
        S.dve(lambda e: e.memset(ones1b.t[:, :], 1.0), [], [ones1b])
        QT2 = [QT, K.sb([128, 32, 128], BF16, "QTb")]
        rec = [K.sb([128, 512], F32, "rec") for _ in range(2)]

        def qprep(n):
            t = n + 2
            p0 = n * 128
            S.dma("sp", q32.t[:, :], P1[t * 128:(t + 1) * 128, 0:4096], writes=[q32])
            S.dma("sp", cosq.t[:, :], K.cosk[p0:p0 + 128, :], writes=[cosq])
            S.dma("sp", sinq.t[:, :], K.sink[p0:p0 + 128, :], writes=[sinq])
            for grp in range(4):
                rope(K, q32.t[:, grp * 1024:(grp + 1) * 1024], q32, kr, tmp, cosq, sinq)
                transpose_blocks(K, kr, 8, QT2[n % 2], 0, eng="dve", dst_off=grp * 8)

        prep_kv(2)
        prep_kv(3)
        qprep(0)
        for n in range(32):
            t = n + 2
            qt = QT2[n % 2]
            aT = aTn[n % 2]
            for kvh in range(8):
                if kvh == 1 and t + 2 <= 33:
                    prep_kv(t + 2)
                if kvh == 4 and n + 1 < 32:
                    qprep(n + 1)
                blks = []
                if t - 1 >= 2:
                    blks.append((KT[(t - 1) % 4], slice(0, 128), VA[(t - 1) % 4], VA[(t - 1) % 4].t[:, kvh, 0:128], MK[0]))
                blks.append((KT[t % 4], slice(0, 128), VA[t % 4], VA[t % 4].t[:, kvh, 0:128], None))
                if t + 1 <= 33:
                    blks.append((KT[(t + 1) % 4], slice(0, 128), VA[(t + 1) % 4], VA[(t + 1) % 4].t[:, kvh, 0:128], MK[1]))
                for tc in range(2):
                    blks.append((KTC, slice(tc * 128, (tc + 1) * 128), VC, VC.t[:, tc, kvh, 0:128], None))
                po = PS[kc % 2]
                pd = PS[2 + kc % 2]
                for bi_, (kt, jc, vt, vap, mk) in enumerate(blks):
                    psx = PS[4 + bi_ % 2]
                    S.pe(lambda e: e.matmul(psx.t[:, :], lhsT=kt.t[:, kvh, jc], rhs=qt.t[:, kvh * 4:(kvh + 1) * 4, :].rearrange("p h i -> p (h i)"),
                                            start=True, stop=True), [kt, qt], [psx])
                    p = p16[pcount % 3]
                    pcount += 1
                    S.act(lambda e: e.activation(out=p.t[:, :], in_=psx.t[:, :], func=AF.Exp, scale=128.0 ** -0.5), [psx], [p])
                    if mk is not None:
                        S.pool(lambda e: e.tensor_tensor(out=p.t[:, :], in0=p.t[:, :], in1=mk.t[:, :], op=ALU.mult), [p, mk], [p])
                    S.pe(lambda e: e.matmul(po.t[:, :], lhsT=vap, rhs=p.t[:, :], start=(bi_ == 0), stop=(bi_ == len(blks) - 1)), [p, vt], [po])
                    S.pe(lambda e: e.matmul(pd.t[:, :], lhsT=ones16.t[:, :], rhs=p.t[:, :], start=(bi_ == 0), stop=False), [p, ones16], [pd])
                S.pe(lambda e: e.matmul(pd.t[:, :], lhsT=ones1b.t[0:1, :], rhs=ESROW.t[0:1, kvh * 4:(kvh + 1) * 4, :].rearrange("p h i -> p (h i)"),
                                        start=False, stop=True), [ESROW, ones1b], [pd])
                r_ = rec[kc % 2]
                kc += 1
                S.dve(lambda e: e.reciprocal(out=r_.t[:, :], in_=pd.t[:, :]), [pd], [r_])
                S.dve(lambda e: e.tensor_tensor(out=aT.t[:, kvh * 4:(kvh + 1) * 4, :],
                                                in0=po.t[:, :].rearrange("p (h i) -> p h i", i=128),
                                                in1=r_.t[:, :].rearrange("p (h i) -> p h i", i=128), op=ALU.mult), [po, r_], [aT])
            S.dma("act", XTv[:, :, t * 128:(t + 1) * 128], aT.t[:, :, :], reads=[aT])
    stage_outproj(K, l, K.c_w_out, list(range(2, NT)))


def stage_final(K):
    S = K.S
    with K.stage():
        fbc = load_bc(K, K.final_norm[0:1, :], D, "fbc")
        xt = [K.sb([128, D], F32, "xt") for _ in range(2)]
        yt = [K.sb([128, D], F32, "yt") for _ in range(2)]
        ss, rs, junk, dg = norm_bufs(K)
        toks = []
        for i, t in enumerate(range(2, NT)):
            x, y = xt[i % 2], yt[i % 2]
            S.dma("sp", x.t[:, :], K.H[t * 128:(t + 1) * 128, :], writes=[x])
            rstd_of(K, x, D, ss, rs, junk, 1.0 / D)
            S.dve(lambda e: e.scalar_tensor_tensor(out=y.t[:, :], in0=x.t[:, :], scalar=rs.t[:, 0:1], in1=fbc.t[:, :], op0=ALU.mult, op1=ALU.mult),
                  [x, rs, fbc], [y])
            toks.append(S.dma("act", K.out[(t - 2) * 128:(t - 1) * 128, :], y.t[:, :], reads=[y]))
        for tk in toks:
            S._wait("act", tk)
            S._wait("sp", tk)

def build(debug=None, upto=9):
    nc = bass.Bass("TRN2", target_bir_lowering=False)
    dt_in = lambda name, shape, dt=F32: nc.dram_tensor(name, list(shape), dt, kind="ExternalInput").ap()
    dbg = debug or ()
    scr = lambda name, shape, dt=F32: nc.dram_tensor(name, list(shape), dt, kind=("ExternalOutput" if name in dbg else "Internal")).ap()
    with ExitStack() as es:
        S = Sched(nc, es)
        K = KB(nc, es, S)
        K.x_in = dt_in("x", [T, D])
        K.ctx_in = dt_in("ctx", [L, D])
        K.c_in = dt_in("c", [D])
        K.c_ctx = dt_in("c_ctx", [D])
        K.mod_w = dt_in("mod_w", [2, D, 6 * D])
        K.mod_b = dt_in("mod_b", [2, 6 * D])
        K.norm_mix = dt_in("norm_mix", [2, D])
        K.norm_ffn = dt_in("norm_ffn", [2, D])
        K.ab_w_in = dt_in("ab_w_in", [D, ABW])
        K.gla_gate_w = dt_in("gla_gate_w", [2, 16, 1024])
        K.gla_gate_b = dt_in("gla_gate_b", [2, 1024])
        K.gla_norm = dt_in("gla_norm", [1, 2048])
        K.sg_norm_w = dt_in("sg_norm_w", [1, 2048])
        K.sg_norm_b = dt_in("sg_norm_b", [1, 2048])
        K.sg_ws = dt_in("sg_ws", [4, 128, 128])
        K.sg_bs = dt_in("sg_bs", [4, 128])
        K.ab_w_out = dt_in("ab_w_out", [D, D])
        K.router_w = dt_in("router_w", [2, D, NEXP])
        K.moe_w_gate = dt_in("moe_w_gate", [2, NEXP, D, FF])
        K.moe_w_up = dt_in("moe_w_up", [2, NEXP, D, FF])
        K.moe_w_down = dt_in("moe_w_down", [2, NEXP, FF, D])
        K.final_norm = dt_in("final_norm", [1, D])
        K.c_w_in = dt_in("c_w_in", [D, 6144])
        K.c_w_out = dt_in("c_w_out", [D, D])
        K.sinks = dt_in("sinks", [1, 32])
        K.cosk = dt_in("cosk", [T, 1024])
        K.sink = dt_in("sink", [T, 1024])
        K.amask = dt_in("amask", [2, 128, 512])
        K.cmat = dt_in("cmat", [6, 128, 128])
        K.cch = dt_in("cch", [128, 2])
        K.ident_in = dt_in("ident", [128, 128])
        K.out = nc.dram_tensor("out", [T, D], F32, kind="ExternalOutput").ap()
        K.MV = scr("MV", [2, 2, 6 * D])
        K.H = scr("H", [NTOK, D])
        K.P0 = scr("P0", [NTOK, ABW])
        K.OG = scr("OG", [2, NTOK, 2048])
        K.HM = scr("HM", [NTOK, D], BF16)
        K.P1 = scr("P1", [NTOK, 6144])
        K.XT = scr("XT", [D, NTOK], BF16)
        K.dbg_attn = "AO" in dbg
        if K.dbg_attn:
            K.AO = scr("AO", [T, D])
            K.RQ = scr("RQ", [T, D])
            K.RK = scr("RK", [T, 1024])
        K.IDXD = scr("IDXD", [2, NEXP, CAP + CAPC], I32)
        K.VALD = scr("VALD", [2, NEXP, CAP + CAPC])
        K.DBG8 = scr("DBG8", [2, 128, 64], I32)
        K.PS = [TT(es.enter_context(nc.psum_tensor("ps%d" % i, [128, 512], F32)), "ps%d" % i) for i in range(8)]
        K.ident = TT(es.enter_context(nc.sbuf_tensor("ident_sb", [128, 128], F32)), "ident")
        K.fm_tmp = TT(es.enter_context(nc.sbuf_tensor("fm_tmp", [32, 128], F32)), "fm_tmp")
        K.lin_pi = 0
        S.dma("sp", K.ident.t[:, :], K.ident_in[:, :], writes=[K.ident])
        S.dma("sp", K.H[0:L, :], K.ctx_in[:, :])
        for i in range(4):
            S.dma("sp", K.H[L + i * 1024:L + (i + 1) * 1024, :], K.x_in[i * 1024:(i + 1) * 1024, :])
        stage_mod(K)
        stage_inproj(K, 0, K.ab_w_in, ABW, K.P0, K.norm_mix[0])
        stage_gla(K)
        stage_merge(K, 0)
        if upto >= 1:
            stage_moe(K, 0, True)
        if upto >= 2:
            stage_inproj(K, 1, K.c_w_in, 6144, K.P1, K.norm_mix[1], ctx_cols=(4096, 6144))
            stage_attn(K, 1)
        if upto >= 3:
            stage_moe(K, 1, False)
        stage_final(K)
        print("instructions:", S.ninst, "sems:", S.nsem)
    return nc


def host_consts():
    j = np.arange(128)[:, None]
    i = np.arange(128)[None, :]
    same = (j // 64) == (i // 64)
    a = -1.0 / 16.0
    A1f = np.where(same & (j <= i), a, 0.0)
    A3f = np.where(same & (j > i), a, 0.0)
    A1b = np.where(same & (j >= i), a, 0.0)
    A3b = np.where(same & (j < i), a, 0.0)
    MKf = np.where(same & (j <= i), 1.0, 0.0)
    MKb = np.where(same & (j >= i), 1.0, 0.0)
    cmat = np.stack([A1f, A3f, A1b, A3b, MKf, MKb]).astype(np.float32)
    cch = np.zeros((128, 2), np.float32)
    cch[:64, 0] = a
    cch[64:, 1] = a
    amask = np.stack([np.tile((j >= i), (1, 4)), np.tile((j <= i), (1, 4))]).astype(np.float32)
    pos = np.arange(T)
    row, col = pos // 64, pos % 64
    inv_freq = (10000.0 ** (-np.arange(0, 64, 2, dtype=np.float32) / 64.0)).astype(np.float32)
    def ang(p):
        a = p.astype(np.float32)[:, None] * inv_freq
        return np.concatenate([a, a], axis=-1)
    an = np.concatenate([ang(row), ang(col)], axis=-1).astype(np.float32)
    cos = np.cos(an).astype(np.float32)
    sin = np.sin(an).astype(np.float32)
    sgn = np.tile(np.concatenate([-np.ones(32), np.ones(32)]), 2).astype(np.float32)
    cosk = np.ascontiguousarray(np.tile(cos, (1, 8)))
    sink = np.ascontiguousarray(np.tile(sin * sgn, (1, 8)))
    return dict(cmat=cmat, cch=cch, ident=np.eye(128, dtype=np.float32), amask=amask, cosk=cosk, sink=sink)


def make_in_map(inp, b):
    f = lambda a: np.ascontiguousarray(a, dtype=np.float32)
    m = dict(
        x=f(inp["x"][b]), ctx=f(inp["ctx"][b]), c=f(inp["c"][b]), c_ctx=f(inp["c_ctx"]),
        mod_w=f(inp["mod_w"]), mod_b=f(inp["mod_b"]), norm_mix=f(inp["norm_mix"]), norm_ffn=f(inp["norm_ffn"]),
        ab_w_in=f(inp["ab_w_in"][0]), gla_gate_w=f(inp["gla_gate_w"][0]), gla_gate_b=f(inp["gla_gate_b"][0]),
        gla_norm=f(inp["gla_norm"]), sg_norm_w=f(inp["sg_norm_w"]), sg_norm_b=f(inp["sg_norm_b"]),
        sg_ws=f(inp["sg_ws"][0]), sg_bs=f(inp["sg_bs"][0]), ab_w_out=f(inp["ab_w_out"][0]),
        router_w=f(inp["router_w"]), moe_w_gate=f(inp["moe_w_gate"]), moe_w_up=f(inp["moe_w_up"]), moe_w_down=f(inp["moe_w_down"]),
        final_norm=f(inp["final_norm"]).reshape(1, D),
        c_w_in=f(inp["c_w_in"][0]), c_w_out=f(inp["c_w_out"][0]), sinks=f(inp["sinks"]).reshape(1, 32),
    )
    m.update(host_consts())
    return m


def kernel(**inputs):
    nc = build()
    in_maps = [make_in_map(inputs, b) for b in range(4)]
    res = run_bass_kernel_spmd(nc, in_maps, core_ids=list(range(4)))
    return np.stack([res.results[b]["out"] for b in range(4)], axis=0)
```
